# Optimizing a Trainium2 kernel written in Bass

```python
import jax, jax.numpy as jnp
from jax import lax
import numpy as np

D_MODEL = 4096
BATCH = 4
SEQ = 4096
DEPTH = 1

ATT_HEADS = 32
ATT_KV_HEADS = 4
ATT_HEAD_DIM = 64
ATT_GROUP = ATT_HEADS // ATT_KV_HEADS
ATT_Q_WIDTH = ATT_HEADS * ATT_HEAD_DIM
ATT_KV_WIDTH = ATT_KV_HEADS * ATT_HEAD_DIM
WINDOW = 128
ATT_BLOCK = 128
RET_HEADS = 8
RET_QK_DIM = 256
RET_V_DIM = 512
RET_QK_WIDTH = RET_HEADS * RET_QK_DIM
RET_V_WIDTH = RET_HEADS * RET_V_DIM
RET_CHUNK = 128
IN_SIZES = (ATT_Q_WIDTH, ATT_KV_WIDTH, ATT_KV_WIDTH, RET_QK_WIDTH, RET_QK_WIDTH, RET_V_WIDTH, RET_V_WIDTH, D_MODEL, D_MODEL)
N_IN = ATT_Q_WIDTH + 2 * ATT_KV_WIDTH + 2 * RET_QK_WIDTH + 2 * RET_V_WIDTH + 2 * D_MODEL
N_EXPERTS = 64
N_GROUPS = 8
EXPERTS_PER_GROUP = N_EXPERTS // N_GROUPS
TOPK_GROUPS = 4
TOP_K = 8
EXPERT_DIM = 512
SHARED_DIM = 512
ROUTED_SCALE = 2.5
DISPATCH_BLOCK = 256
EPS = 1e-6

kernel_name = "hybrid_swa_sink_retention_moe_adaln"


def rms_norm(x, g):
    xf = x.astype(jnp.float32)
    y = xf * lax.rsqrt(jnp.mean(xf * xf, axis=-1, keepdims=True) + EPS)
    return (y * g.astype(jnp.float32)).astype(x.dtype)


def modulate(h, shift, scale):
    return h * (1.0 + scale[:, None, :]) + shift[:, None, :]


def alibi_slopes(n):
    return jnp.exp2(-8.0 * jnp.arange(1, n + 1, dtype=jnp.float32) / n)


def sliding_window_attention(q, k, v, sinks):
    b, s = q.shape[0], q.shape[1]
    nb = s // ATT_BLOCK
    qb = q.reshape(b, nb, ATT_BLOCK, ATT_KV_HEADS, ATT_GROUP, ATT_HEAD_DIM)

    def band(t):
        tb = t.reshape(b, nb, ATT_BLOCK, ATT_KV_HEADS, ATT_HEAD_DIM)
        prev = jnp.pad(tb, ((0, 0), (1, 0), (0, 0), (0, 0), (0, 0)))[:, :-1]
        return jnp.concatenate([prev, tb], axis=2)

    kb, vb = band(k), band(v)
    scores = jnp.einsum('bnqhgd,bnkhd->bnhgqk', qb, kb).astype(jnp.float32) * (ATT_HEAD_DIM ** -0.5)
    qi = jnp.arange(ATT_BLOCK)[:, None]
    kj = jnp.arange(2 * ATT_BLOCK)[None, :]
    dist = qi + ATT_BLOCK - kj
    key_pos = jnp.arange(nb)[:, None, None] * ATT_BLOCK - ATT_BLOCK + kj[None]
    valid = (dist >= 0) & (dist < WINDOW) & (key_pos >= 0)
    slopes = alibi_slopes(ATT_HEADS).reshape(ATT_KV_HEADS, ATT_GROUP)
    scores = scores - slopes[:, :, None, None] * dist.astype(jnp.float32)
    scores = jnp.where(valid[None, :, None, None], scores, -jnp.inf)
    sink = sinks.astype(jnp.float32).reshape(ATT_KV_HEADS, ATT_GROUP)[:, :, None, None]
    m = jnp.maximum(scores.max(-1, keepdims=True), sink)
    p = jnp.exp(scores - m)
    probs = p / (p.sum(-1, keepdims=True) + jnp.exp(sink - m))
    out = jnp.einsum('bnhgqk,bnkhd->bnqhgd', probs.astype(v.dtype), vb)
    return out.reshape(b, s, ATT_Q_WIDTH)


def retention(q, k, v):
    b, s = q.shape[0], q.shape[1]
    n = s // RET_CHUNK
    log_g = jnp.log1p(-jnp.exp2(-5.0 - jnp.arange(RET_HEADS, dtype=jnp.float32)))
    pos = jnp.arange(RET_CHUNK, dtype=jnp.float32)
    rel = pos[:, None] - pos[None, :]
    decay_mask = jnp.where(rel[None] >= 0, jnp.exp(rel[None] * log_g[:, None, None]), 0.0)
    q_decay = jnp.exp((pos[:, None] + 1.0) * log_g[None])
    k_decay = jnp.exp((RET_CHUNK - 1.0 - pos[:, None]) * log_g[None])
    chunk_decay = jnp.exp(RET_CHUNK * log_g)

    def chunks(t):
        return t.astype(jnp.float32).reshape(b, n, RET_CHUNK, RET_HEADS, t.shape[-1]).swapaxes(0, 1)

    qc, kc, vc = chunks(q), chunks(k * (RET_QK_DIM ** -0.5)), chunks(v)

    def step(state, xs):
        qi, ki, vi = xs
        attn = jnp.einsum('bihd,bjhd->bhij', qi, ki) * decay_mask
        intra = jnp.einsum('bhij,bjhe->bihe', attn, vi)
        inter = jnp.einsum('bihd,bhde->bihe', qi, state) * q_decay[None, :, :, None]
        state = state * chunk_decay[None, :, None, None] + jnp.einsum('bjhd,bjhe->bhde', ki * k_decay[None, :, :, None], vi)
        return state, intra + inter

    state0 = jnp.zeros((b, RET_HEADS, RET_QK_DIM, RET_V_DIM), jnp.float32)
    _, o = lax.scan(step, state0, (qc, kc, vc))
    o = o.swapaxes(0, 1).reshape(b, s, RET_HEADS, RET_V_DIM)
    mu = o.mean(-1, keepdims=True)
    var = jnp.mean(jnp.square(o - mu), axis=-1, keepdims=True)
    return (o - mu) * lax.rsqrt(var + EPS)


def swiglu(h, wg, wu, wd):
    return (jax.nn.silu(h @ wg) * (h @ wu)) @ wd


def route(h, w_router, b_router):
    t = h.shape[0]
    scores = jax.nn.sigmoid((h @ w_router).astype(jnp.float32))
    choice = scores + b_router.astype(jnp.float32)
    grp = choice.reshape(t, N_GROUPS, EXPERTS_PER_GROUP)
    grp_score = lax.top_k(grp, 2)[0].sum(-1)
    _, grp_idx = lax.top_k(grp_score, TOPK_GROUPS)
    grp_mask = jax.nn.one_hot(grp_idx, N_GROUPS, dtype=jnp.float32).sum(1) > 0
    expert_mask = jnp.repeat(grp_mask, EXPERTS_PER_GROUP, axis=-1)
    _, idx = lax.top_k(jnp.where(expert_mask, choice, -jnp.inf), TOP_K)
    w = jnp.take_along_axis(scores, idx, axis=-1)
    w = w / w.sum(-1, keepdims=True) * ROUTED_SCALE
    return idx, w


def routed_experts(h, idx, w, w_gate, w_up, w_down):
    t, d = h.shape
    a = t * TOP_K
    flat_e = idx.reshape(a)
    flat_w = w.reshape(a).astype(h.dtype)
    order = jnp.argsort(flat_e)
    sorted_e = flat_e[order]
    tok = (order // TOP_K).astype(jnp.int32)
    counts = jnp.bincount(flat_e, length=N_EXPERTS)
    padded = (counts + DISPATCH_BLOCK - 1) // DISPATCH_BLOCK * DISPATCH_BLOCK
    pad_end = jnp.cumsum(padded)
    pad_start = pad_end - padded
    start = jnp.cumsum(counts) - counts
    dest = pad_start[sorted_e] + (jnp.arange(a) - start[sorted_e])
    n_blocks = (a + DISPATCH_BLOCK - 1) // DISPATCH_BLOCK + N_EXPERTS
    p = n_blocks * DISPATCH_BLOCK
    buf_tok = jnp.full((p,), t, jnp.int32).at[dest].set(tok)
    buf_w = jnp.zeros((p,), h.dtype).at[dest].set(flat_w[order])
    blk_e = jnp.minimum(jnp.searchsorted(pad_end, jnp.arange(n_blocks) * DISPATCH_BLOCK, side='right'), N_EXPERTS - 1)
    h_pad = jnp.concatenate([h, jnp.zeros((1, d), h.dtype)], axis=0)

    def body(acc, xs):
        tok_b, w_b, e = xs
        y = swiglu(h_pad[tok_b], w_gate[e], w_up[e], w_down[e])
        return acc.at[tok_b].add(y * w_b[:, None]), None

    acc0 = jnp.zeros((t + 1, d), h.dtype)
    acc, _ = lax.scan(body, acc0, (buf_tok.reshape(n_blocks, DISPATCH_BLOCK), buf_w.reshape(n_blocks, DISPATCH_BLOCK), blk_e))
    return acc[:t]


def setup_inputs(seed: int = 0) -> dict:
    key = jax.random.key(seed)
    ks = jax.random.split(key, 22)
    f32 = jnp.float32

    def nrm(k, shape, fan_in, s=1.0):
        return (s * fan_in ** -0.5) * jax.random.normal(k, shape, f32)

    D = D_MODEL
    return {
        "x": jax.random.normal(ks[0], (BATCH, SEQ, D), f32),
        "c": jax.random.normal(ks[1], (BATCH, D), f32),
        "w_ada": nrm(ks[2], (DEPTH, D, 6 * D), D, 0.5),
        "b_ada": 0.02 * jax.random.normal(ks[3], (DEPTH, 6 * D), f32),
        "g_norm_mix": 1.0 + 0.05 * jax.random.normal(ks[4], (DEPTH, D), f32),
        "w_in": nrm(ks[5], (DEPTH, D, N_IN), D),
        "attn_sinks": 0.5 * jax.random.normal(ks[6], (DEPTH, ATT_HEADS), f32),
        "w_attn_out": nrm(ks[7], (DEPTH, ATT_Q_WIDTH, D), ATT_Q_WIDTH),
        "w_ret_out": nrm(ks[8], (DEPTH, RET_V_WIDTH, D), RET_V_WIDTH),
        "w_o": nrm(ks[9], (DEPTH, D, D), D),
        "g_norm_ffn": 1.0 + 0.05 * jax.random.normal(ks[10], (DEPTH, D), f32),
        "w_router": nrm(ks[11], (DEPTH, D, N_EXPERTS), D),
        "b_router": 0.01 * jax.random.normal(ks[12], (DEPTH, N_EXPERTS), f32),
        "w_gate": nrm(ks[13], (DEPTH, N_EXPERTS, D, EXPERT_DIM), D),
        "w_up": nrm(ks[14], (DEPTH, N_EXPERTS, D, EXPERT_DIM), D),
        "w_down": nrm(ks[15], (DEPTH, N_EXPERTS, EXPERT_DIM, D), EXPERT_DIM),
        "w_sh_gate": nrm(ks[16], (DEPTH, D, SHARED_DIM), D),
        "w_sh_up": nrm(ks[17], (DEPTH, D, SHARED_DIM), D),
        "w_sh_down": nrm(ks[18], (DEPTH, SHARED_DIM, D), SHARED_DIM),
        "g_norm_final": 1.0 + 0.05 * jax.random.normal(ks[19], (D,), f32),
    }


def reference(x, c, w_ada, b_ada, g_norm_mix, w_in, attn_sinks, w_attn_out, w_ret_out, w_o, g_norm_ffn, w_router, b_router, w_gate, w_up, w_down, w_sh_gate, w_sh_up, w_sh_down, g_norm_final):
    b, s, d = x.shape
    offsets = []
    acc_off = 0
    for size in IN_SIZES[:-1]:
        acc_off += size
        offsets.append(acc_off)
    cond = jax.nn.silu(c)
    for l in range(DEPTH):
        mod = cond @ w_ada[l] + b_ada[l]
        sh1, sc1, gt1, sh2, sc2, gt2 = jnp.split(mod, 6, axis=-1)

        h = modulate(rms_norm(x, g_norm_mix[l]), sh1, sc1)
        proj = h @ w_in[l]
        qa, ka, va, qr, kr, vr, gr, gate_a, gate_b = jnp.split(proj, offsets, axis=-1)
        ya = sliding_window_attention(
            qa.reshape(b, s, ATT_HEADS, ATT_HEAD_DIM),
            ka.reshape(b, s, ATT_KV_HEADS, ATT_HEAD_DIM),
            va.reshape(b, s, ATT_KV_HEADS, ATT_HEAD_DIM),
            attn_sinks[l]) @ w_attn_out[l]
        ret = retention(
            qr.reshape(b, s, RET_HEADS, RET_QK_DIM),
            kr.reshape(b, s, RET_HEADS, RET_QK_DIM),
            vr.reshape(b, s, RET_HEADS, RET_V_DIM))
        yr = (jax.nn.silu(gr.astype(jnp.float32)) * ret.reshape(b, s, RET_V_WIDTH)).astype(x.dtype) @ w_ret_out[l]
        mix = jax.nn.sigmoid(gate_a) * ya + jax.nn.sigmoid(gate_b) * yr
        x = x + gt1[:, None, :] * (mix @ w_o[l])

        h2 = modulate(rms_norm(x, g_norm_ffn[l]), sh2, sc2).reshape(b * s, d)
        idx, wts = route(h2, w_router[l], b_router[l])
        y = routed_experts(h2, idx, wts, w_gate[l], w_up[l], w_down[l]) + swiglu(h2, w_sh_gate[l], w_sh_up[l], w_sh_down[l])
        x = x + gt2[:, None, :] * y.reshape(b, s, d)
    return rms_norm(x, g_norm_final)
```

```python
import numpy as np
from contextlib import ExitStack
import ml_dtypes
import concourse.bass as bass
import concourse.mybir as mybir
from concourse.bass_utils import run_bass_kernel_spmd

F32 = mybir.dt.float32
BF16 = mybir.dt.bfloat16
AF = mybir.ActivationFunctionType
ALU = mybir.AluOpType
AX = mybir.AxisListType

D = 4096
KC = 32
NCORE = 4
N_IN = 23040
EPS = 1e-6
BIG = 1.0e6
KG = 8
NWB = 4


class Buf:
    __slots__ = ("name", "lw", "rd")

    def __init__(self, name):
        self.name = name
        self.lw = None
        self.rd = {}


class Prog:
    ENG = ("pe", "act", "dve", "pool", "sp")

    def __init__(self, nc, es):
        self.nc = nc
        self.es = es
        self.streams = {e: [] for e in self.ENG}
        self.sems = {}
        self.cnt = {}
        self.known = {e: {} for e in self.ENG}
        for e in self.ENG:
            self._sem("E_" + e)

    def _sem(self, key):
        if key not in self.sems:
            self.sems[key] = self.es.enter_context(self.nc.semaphore(key))
            self.cnt[key] = 0
        return self.sems[key]

    def op(self, eng, fn, reads=(), writes=(), dma=None):
        deps = []
        for b in reads:
            if b.lw:
                deps.append(b.lw)
        for b in writes:
            if b.lw:
                deps.append(b.lw)
            deps.extend(b.rd.items())
        if dma is None:
            key, inc = "E_" + eng, 1
        else:
            key, inc = "D_" + dma, 16
            self._sem(key)
        waits = {}
        kn = self.known[eng]
        for k, v in deps:
            if k == "E_pe" and eng == "pe":
                continue
            if kn.get(k, 0) >= v:
                continue
            if waits.get(k, 0) < v:
                waits[k] = v
        for k, v in waits.items():
            kn[k] = v
        self.cnt[key] += inc
        done = (key, self.cnt[key])
        self.streams[eng].append((fn, list(waits.items()), key, inc))
        for b in writes:
            b.lw = done
            b.rd = {}
        for b in reads:
            if b not in writes:
                if b.rd.get(key, 0) < done[1]:
                    b.rd[key] = done[1]
        return done

    def barrier(self):
        allc = [(k, v) for k, v in self.cnt.items() if v > 0]
        for e in self.ENG:
            w = [(k, v) for k, v in allc if self.known[e].get(k, 0) < v and not (k == "E_" + e)]
            for k, v in w:
                self.known[e][k] = v
            self.streams[e].append((None, w, None, 0))

    def emit(self):
        nc = self.nc
        sems = self.sems
        streams = self.streams
        with nc.Block() as block:
            def mk(e):
                def body(eng):
                    for fn, waits, key, inc in streams[e]:
                        for k, v in waits:
                            eng.wait_ge(sems[k], v)
                        if fn is not None:
                            fn(eng).then_inc(sems[key], inc)
                return body
            block.tensor(mk("pe"))
            block.scalar(mk("act"))
            block.vector(mk("dve"))
            block.gpsimd(mk("pool"))
            block.sync(mk("sp"))


class _Cut(Exception):
    pass


def build_program(nseq=32, stage="full", cutn=0):
    nc = bass.Bass("TRN2", target_bir_lowering=False)
    es = ExitStack()
    P = Prog(nc, es)
    full = stage == "full"

    def din(name, shape, dt=F32):
        return nc.dram_tensor(name, list(shape), dt, kind="ExternalInput").ap()

    def dint(name, shape, dt=F32):
        return nc.dram_tensor(name, list(shape), dt, kind="Internal").ap()

    def sb(name, shape, dt=F32):
        return es.enter_context(nc.sbuf_tensor("s_" + name, list(shape), dt))

    def ps(name, shape, dt=F32):
        return es.enter_context(nc.psum_tensor("p_" + name, list(shape), dt))

    x_seq = din("x_seq", [nseq * 128, D])
    c_fm = din("c_fm", [128, KC])
    dm0_in = din("dm0", [128, 256]); dmg_in = din("dmg", [128, 256])
    dmask_in = din("dmask", [128, 8 * 128]); qd_in = din("qd", [128, 8 * 128]); kdec_in = din("kdec", [128, 8])
    identb_in = din("identb", [128, 128], BF16); identf_in = din("identf", [128, 128])
    b_ada_in = din("b_ada", [1, 6 * D]); b_ada_fm = din("b_ada_fm", [128, 6 * KC])
    g_mix_fm = din("g_mix_fm", [128, KC]); g_ffn_fm = din("g_ffn_fm", [128, KC])
    g_fin_in = din("g_fin", [1, D]); sinks_in = din("sinks", [1, 32]); b_router_in = din("b_router", [1, 64])
    W = {"w_ada": din("w_ada", [D, 6 * D]), "w_in": din("w_in", [D, N_IN]), "w_ao": din("w_ao", [2048, D]),
         "w_ro": din("w_ro", [D, D]), "w_o": din("w_o", [D, D]), "w_router": din("w_router", [D, 64])}
    if full:
        W.update({"w_gate": din("w_gate", [64, D, 512]), "w_up": din("w_up", [64, D, 512]), "w_down": din("w_down", [64, 512, D]),
                  "w_sg": din("w_sg", [D, 512]), "w_su": din("w_su", [D, 512]), "w_sd": din("w_sd", [512, D])})
    mod_d = dint("mod_d", [1, 6 * D])
    x1_d = dint("x1_d", [nseq * 128, D])
    h2T_d = dint("h2T_d", [128, KC, nseq * 128], BF16)
    rw_d = dint("rw_d", [128, nseq, 64])
    out_d = nc.dram_tensor("out", [nseq * 128, D], F32, kind="ExternalOutput").ap()

    identb = sb("identb", [128, 128], BF16); identf = sb("identf", [128, 128])
    dm0 = sb("dm0", [128, 256]); dmg = sb("dmg", [128, 256])
    dmask = sb("dmask", [128, 8, 128]); qd = sb("qd", [128, 8, 128]); kdec = sb("kdec", [128, 8])
    cfm = sb("cfm", [128, KC]); cbf = sb("cbf", [128, KC], BF16)
    gmix = sb("gmix", [128, KC]); gffn = sb("gffn", [128, KC])
    modfm = sb("modfm", [128, 6 * KC]); bfm = sb("bfm", [128, 6 * KC])
    G1 = sb("G1", [128, KC]); G2 = sb("G2", [128, KC])
    sinkexp = sb("sinkexp", [128, 32]); brbc = sb("brbc", [128, 64])
    wr_sb = sb("wr_sb", [128, KC, 64])
    Bc = Buf("consts"); Bmod = Buf("mod")
    for t, src in [(identb, identb_in), (identf, identf_in), (dm0, dm0_in), (dmg, dmg_in), (dmask, dmask_in), (qd, qd_in),
                   (kdec, kdec_in), (cfm, c_fm), (gmix, g_mix_fm), (gffn, g_ffn_fm), (bfm, b_ada_fm)]:
        tv = t[:].rearrange("p a b -> p (a b)") if len(t.shape) == 3 else t[:]
        P.op("sp", lambda e, tv=tv, src=src: e.dma_start(out=tv, in_=src), writes=[Bc], dma="c")
    P.op("sp", lambda e: e.dma_start(out=sinkexp[:], in_=sinks_in.partition_broadcast(128)), writes=[Bc], dma="c")
    P.op("sp", lambda e: e.dma_start(out=brbc[:], in_=b_router_in.partition_broadcast(128)), writes=[Bc], dma="c")
    P.op("sp", lambda e: e.dma_start(out=wr_sb[:], in_=W["w_router"].rearrange("(kc p) n -> p kc n", p=128)), writes=[Bc], dma="c")
    P.op("act", lambda e: e.activation(out=sinkexp[:], in_=sinkexp[:], func=AF.Exp), reads=[Bc], writes=[Bc])
    P.op("act", lambda e: e.activation(out=cbf[:], in_=cfm[:], func=AF.Silu), reads=[Bc], writes=[Bmod])

    pA = [ps("pA%d" % i, [128, 512]) for i in range(2)]; BpA = [Buf("pA%d" % i) for i in range(2)]
    pT = ps("pT", [128, 1024], BF16); BpT = Buf("pT")
    pF = ps("pF", [128, 512]); BpF = Buf("pF")
    pS = ps("pS", [128, 512]); BpS = Buf("pS")
    pO = ps("pO", [128, 512]); BpO = Buf("pO")
    pU = [ps("pU%d" % i, [128, 512]) for i in range(2)]; BpU = [Buf("pU%d" % i) for i in range(2)]
    pa_i = [0]

    def nextA():
        i = pa_i[0]
        pa_i[0] ^= 1
        return pA[i], BpA[i]

    wb = [sb("wb%d" % i, [128, KG, 512], BF16) for i in range(NWB)]
    Bwb = [Buf("wb%d" % i) for i in range(NWB)]
    wb_i = [0]

    SCR_N = 240
    scr_pools = []
    scr_map = {}

    def scratch_for(key):
        if key not in scr_map:
            idx = len(scr_map)
            if idx % SCR_N == 0:
                scr_pools.append(dint("scr%d" % (idx // SCR_N), [SCR_N, 128, KG * 512], BF16))
            scr_map[key] = (scr_pools[-1][idx % SCR_N], Buf("scr%d" % idx), False)
        return scr_map[key]

    def load_raw(key, first_fn):
        i = wb_i[0]
        wb_i[0] = (i + 1) % NWB
        flat = wb[i][:].rearrange("p a b -> p (a b)")
        if key is None:
            P.op("pool", lambda e, i=i: first_fn(e, i), writes=[Bwb[i]], dma="wb%d" % i)
            return wb[i], Bwb[i]
        ap, Bs, seen = scratch_for(key)
        if not seen:
            P.op("pool", lambda e, i=i: first_fn(e, i), writes=[Bwb[i]], dma="wb%d" % i)
            P.op("sp", lambda e, ap=ap, flat=flat: e.dma_start(out=ap, in_=flat), reads=[Bwb[i]], writes=[Bs], dma="scrw%d" % i)
            scr_map[key] = (ap, Bs, True)
        else:
            P.op("pool", lambda e, ap=ap, flat=flat: e.dma_start(out=flat, in_=ap), reads=[Bs], writes=[Bwb[i]], dma="wb%d" % i)
        return wb[i], Bwb[i]

    def load_tile(view, nk=KG, ncol=512, key=None):
        assert key is None or (nk == KG and ncol == 512)
        v = view.rearrange("(kc p) n -> p kc n", p=128)
        return load_raw(key, lambda e, i: e.dma_start(out=wb[i][:, 0:nk, 0:ncol], in_=v))

    def proj(outs, lhs_list, src, nk, ncol=512, key=None):
        ng = nk // KG
        for g in range(ng):
            w, Bwt = load_tile(src[g * KG * 128:(g + 1) * KG * 128, :], KG, ncol, key=None if key is None else key + (g,))
            for (outp, Bout), (actT, Bact, cs) in zip(outs, lhs_list):
                def fn(e, g=g, w=w, outp=outp, actT=actT, cs=cs):
                    ins = None
                    for k in range(KG):
                        kc = g * KG + k
                        ins = e.matmul(outp[:, 0:ncol], actT[:, kc, cs], w[:, k, 0:ncol], start=(kc == 0), stop=(kc == nk - 1))
                    return ins
                P.op("pe", fn, reads=[Bact, Bwt], writes=[Bout])

    XA = sb("XA", [128, D]); BXA = Buf("XA")
    XB = sb("XB", [128, D]); BXB = Buf("XB")
    gbc = sb("gbc", [128, D]); Bgbc = Buf("gbc")
    S32 = sb("S32", [128, 8, 2, 512]); BS32 = [Buf("S32_%d" % h) for h in range(8)]
    hT = sb("hT", [128, KC, 128], BF16); BhT = Buf("hT")
    aoT = sb("aoT", [128, 16, 128], BF16); BaoT = Buf("aoT")
    roT = sb("roT", [128, KC, 128], BF16); BroT = Buf("roT")
    rotm = sb("rotm", [128, D], BF16); Brotm = Buf("rotm")
    st1 = sb("st1", [128, 8]); Bst = Buf("st1")

    mrow = [sb("mrow%d" % i, [1, 512]) for i in range(2)]; Bmrow = [Buf("mrow%d" % i) for i in range(2)]
    Bmd = Buf("mod_d")
    for cb in range(48):
        pp, Bpp = nextA()
        src = W["w_ada"][:, cb * 512:(cb + 1) * 512]
        for g in range(KC // KG):
            w, Bwt = load_tile(src[g * KG * 128:(g + 1) * KG * 128, :])

            def fn(e, g=g, w=w, pp=pp):
                ins = None
                for k in range(KG):
                    kc = g * KG + k
                    ins = e.matmul(pp[0:1, :], cbf[:, kc:kc + 1], w[:, k, :], start=(kc == 0), stop=(kc == KC - 1))
                return ins
            P.op("pe", fn, reads=[Bmod, Bwt], writes=[Bpp])
        mi = cb % 2
        P.op("dve", lambda e, pp=pp, mi=mi: e.tensor_copy(out=mrow[mi][:], in_=pp[0:1, :]), reads=[Bpp], writes=[Bmrow[mi]])
        P.op("sp", lambda e, mi=mi, cb=cb: e.dma_start(out=mod_d[:, cb * 512:(cb + 1) * 512], in_=mrow[mi][:]),
             reads=[Bmrow[mi]], writes=[Bmd], dma="modo")

        def fn(e, mi=mi):
            ins = None
            for j in range(4):
                ins = e.transpose(pF[:, j:j + 1], mrow[mi][0:1, j * 128:(j + 1) * 128], identf[0:1, 0:1])
            return ins
        P.op("pe", fn, reads=[Bmrow[mi], Bc], writes=[BpF])
        P.op("dve", lambda e, cb=cb: e.tensor_copy(out=modfm[:, cb * 4:(cb + 1) * 4], in_=pF[:, 0:4]), reads=[BpF], writes=[Bmod])
    P.op("dve", lambda e: e.tensor_tensor(out=modfm[:], in0=modfm[:], in1=bfm[:], op=ALU.add), reads=[Bc], writes=[Bmod])
    P.op("dve", lambda e: e.scalar_tensor_tensor(out=G1[:], in0=modfm[:, 32:64], scalar=1.0, in1=gmix[:], op0=ALU.add, op1=ALU.mult),
         reads=[Bc], writes=[Bmod])
    P.op("dve", lambda e: e.scalar_tensor_tensor(out=G2[:], in0=modfm[:, 128:160], scalar=1.0, in1=gffn[:], op0=ALU.add, op1=ALU.mult),
         reads=[Bc], writes=[Bmod])
    SH1 = modfm[:, 0:32]
    SH2 = modfm[:, 96:128]
    P.op("sp", lambda e: e.dma_start(out=gbc[:], in_=mod_d[:, 2 * D:3 * D].partition_broadcast(128)), reads=[Bmd], writes=[Bgbc], dma="gbc")
    P.op("sp", lambda e: e.dma_start(out=XB[:], in_=b_ada_in[:, 2 * D:3 * D].partition_broadcast(128)), writes=[BXB], dma="xb")
    P.op("dve", lambda e: e.tensor_tensor(out=gbc[:], in0=gbc[:], in1=XB[:], op=ALU.add), reads=[BXB], writes=[Bgbc])

    kaT = [sb("kaT%d" % i, [64, 4, 128], BF16) for i in range(2)]; BkaT = [Buf("kaT%d" % i) for i in range(2)]
    vau = [sb("vau%d" % i, [128, 4, 72], BF16) for i in range(2)]; Bvau = [Buf("vau%d" % i) for i in range(2)]
    kvtm = sb("kvtm", [128, 256], BF16); Bkvtm = Buf("kvtm")
    qtm = sb("qtm", [128, 512], BF16); Bqtm = Buf("qtm")
    qT = sb("qT", [64, 8, 128], BF16); BqT = Buf("qT")
    sT = sb("sT", [128, 256]); BsT = Buf("sT")
    eT = sb("eT", [128, 256], BF16); BeT = Buf("eT")
    rden = sb("rden", [128, 2]); Brden = Buf("rden")
    rq = sb("rq", [128, 512], BF16); Brq = Buf("rq")
    rk = sb("rk", [128, 512], BF16); Brk = Buf("rk")
    rv = sb("rv", [128, 512], BF16); Brv = Buf("rv")
    rg = sb("rg", [128, 512], BF16); Brg = Buf("rg")
    rqT = sb("rqT", [128, 2, 128], BF16); BrqT = Buf("rqT")
    rqdT = sb("rqdT", [128, 2, 128], BF16); BrqdT = Buf("rqdT")
    rkT = sb("rkT", [128, 2, 128], BF16); BrkT = Buf("rkT")
    rkd = sb("rkd", [128, 256], BF16); Brkd = Buf("rkd")
    adT = sb("adT", [128, 128], BF16); BadT = Buf("adT")
    Sbfh = sb("Sbfh", [128, 2, 512], BF16); BSbfh = Buf("Sbfh")
    bnst = sb("bnst", [128, 6]); bnag = sb("bnag", [128, 2]); Bbn = Buf("bn")
    otmp = sb("otmp", [128, 512]); Botmp = Buf("otmp")
    sga = sb("sga", [128, 512]); Bsga = Buf("sga")
    sgb = sb("sgb", [128, 512]); Bsgb = Buf("sgb")
    h32g = sb("h32g", [128, 4, 128]); Bh32g = Buf("h32g")
    rt = {n: sb("rt_" + n, [128, 64]) for n in ("sc", "ch", "t1", "t2", "cm", "rw")}
    rt8 = {n: sb("rt8_" + n, [128, 8]) for n in ("m1", "m2", "gs", "top", "gm", "mx")}
    Brt = Buf("rt")
    Bx1d = Buf("x1_d"); Bh2d = Buf("h2T_d"); Brwd = Buf("rw_d")

    for h in range(8):
        P.op("dve", lambda e, h=h: e.memset(S32[:, h].rearrange("p a b -> p (a b)"), 0.0), writes=[BS32[h]])
    for i in range(2):
        P.op("dve", lambda e, i=i: e.memset(vau[i][:].rearrange("p a b -> p (a b)"), 0.0), writes=[Bvau[i]])
        P.op("dve", lambda e, i=i: e.memset(vau[i][:, :, 64:65], 1.0), writes=[Bvau[i]])
        P.op("dve", lambda e, i=i: e.memset(kaT[i][:].rearrange("p a b -> p (a b)"), 0.0), writes=[BkaT[i]])

    slopes = [float(2.0 ** (-8.0 * (h + 1) / 32)) for h in range(32)]
    gamC = [float((1.0 - 2.0 ** (-5.0 - h)) ** 128) for h in range(8)]

    def transposes_bf(src, Bsrc, ncol, dst_fn, Bdst):
        nch = ncol // 128
        for g0 in range(0, nch, 8):
            n = min(8, nch - g0)

            def fn(e, g0=g0, n=n):
                ins = None
                for j in range(n):
                    ins = e.transpose(pT[:, j * 128:(j + 1) * 128], src[:, (g0 + j) * 128:(g0 + j + 1) * 128], identb[:])
                return ins
            P.op("pe", fn, reads=[Bsrc, Bc], writes=[BpT])
            for j in range(n):
                if j % 2:
                    P.op("act", lambda e, j=j, g0=g0: e.copy(out=dst_fn(g0 + j), in_=pT[:, j * 128:(j + 1) * 128]), reads=[BpT], writes=[Bdst])
                else:
                    P.op("dve", lambda e, j=j, g0=g0: e.tensor_copy(out=dst_fn(g0 + j), in_=pT[:, j * 128:(j + 1) * 128]), reads=[BpT], writes=[Bdst])

    def rms_to_T(xin, Bxin, xnorm, Bxnorm, Gs, Hs, dstb, Bdstb, router=False):
        P.op("act", lambda e: e.activation(out=rotm[:], in_=xin[:], func=AF.Square, accum_out=st1[:, 0:1]),
             reads=[Bxin], writes=[Brotm, Bst])
        P.op("dve", lambda e: e.tensor_scalar(out=st1[:, 1:2], in0=st1[:, 0:1], scalar1=1.0 / D, scalar2=EPS, op0=ALU.mult, op1=ALU.add),
             writes=[Bst])
        P.op("act", lambda e: e.activation(out=st1[:, 3:4], in_=st1[:, 1:2], func=AF.Sqrt), writes=[Bst])
        P.op("dve", lambda e: e.reciprocal(out=st1[:, 2:3], in_=st1[:, 3:4]), writes=[Bst])
        P.op("dve", lambda e: e.tensor_scalar(out=xnorm[:], in0=xin[:], scalar1=st1[:, 2:3], scalar2=None, op0=ALU.mult),
             reads=[Bxin, Bst], writes=[Bxnorm])
        for g0 in range(0, KC, 4):
            def fn(e, g0=g0):
                ins = None
                for j in range(4):
                    ins = e.transpose(pF[:, j * 128:(j + 1) * 128], xnorm[:, (g0 + j) * 128:(g0 + j + 1) * 128], identf[:])
                return ins
            P.op("pe", fn, reads=[Bxnorm, Bc], writes=[BpF])
            for j in range(4):
                kc = g0 + j
                P.op("act", lambda e, j=j, kc=kc: e.activation(out=dstb[:, kc, :], in_=pF[:, j * 128:(j + 1) * 128], func=AF.Identity,
                                                              bias=Hs[:, kc:kc + 1], scale=Gs[:, kc:kc + 1]),
                     reads=[BpF, Bmod], writes=[Bdstb])
                if router:
                    P.op("act", lambda e, j=j, kc=kc: e.activation(out=h32g[:, j, :], in_=pF[:, j * 128:(j + 1) * 128], func=AF.Identity,
                                                                  bias=Hs[:, kc:kc + 1], scale=Gs[:, kc:kc + 1]),
                         reads=[BpF, Bmod], writes=[Bh32g])
            if router:
                def fn(e, g0=g0):
                    ins = None
                    for j in range(4):
                        kc = g0 + j
                        ins = e.matmul(pS[:, 0:64], h32g[:, j, :], wr_sb[:, kc, :], start=(kc == 0), stop=(kc == KC - 1))
                    return ins
                P.op("pe", fn, reads=[Bh32g, Bc], writes=[BpS])

    def wcols(key, c0, n=512):
        return W[key][:, c0:c0 + n]

    def cut(n, buf, B):
        if cutn == n:
            P.op("sp", lambda e: e.dma_start(out=out_d[0:128, :], in_=buf[:]), reads=[B], dma="out")
            raise _Cut()

    def _body():
        cut(1, gbc, Bgbc)
        for tg in range(nseq):
            cur = tg % 2
            prv = 1 - cur
            hl = (hT, BhT, slice(0, 128))
            P.op("sp", lambda e, tg=tg: e.dma_start(out=XA[:], in_=x_seq[tg * 128:(tg + 1) * 128, :]), writes=[BXA], dma="xa")
            rms_to_T(XA, BXA, XB, BXB, G1, SH1, hT, BhT)
            cut(2, XB, BXB)
            pp, Bpp = nextA()
            proj([(pp, Bpp)], [hl], wcols("w_in", 2048), KC, key=("in", 4))
            P.op("act", lambda e, pp=pp: e.copy(out=kvtm[:], in_=pp[:, 0:256]), reads=[Bpp], writes=[Bkvtm])
            cut(311, XB, BXB)
            for g in range(4):
                P.op("act", lambda e, pp=pp, cur=cur, g=g: e.copy(out=vau[cur][:, g, 0:64], in_=pp[:, 256 + g * 64:256 + (g + 1) * 64]),
                     reads=[Bpp], writes=[Bvau[cur]])
            cut(312, XB, BXB)

            def fn(e):
                ins = None
                for g in range(4):
                    ins = e.transpose(pT[0:64, g * 128:(g + 1) * 128], kvtm[:, g * 64:(g + 1) * 64], identb[:])
                return ins
            P.op("pe", fn, reads=[Bkvtm, Bc], writes=[BpT])
            cut(313, XB, BXB)
            P.op("dve", lambda e, cur=cur: e.tensor_copy(out=kaT[cur][:].rearrange("p g t -> p (g t)"), in_=pT[0:64, 0:512]),
                 reads=[BpT], writes=[BkaT[cur]])
            cut(31, XB, BXB)
            dmt = dm0 if tg == 0 else dmg
            for g in range(4):
                pp, Bpp = nextA()
                proj([(pp, Bpp)], [hl], wcols("w_in", g * 512), KC, key=("in", g))
                P.op("act", lambda e, pp=pp: e.copy(out=qtm[:], in_=pp[:]), reads=[Bpp], writes=[Bqtm])

                def fn(e):
                    ins = None
                    for hh_ in range(8):
                        ins = e.transpose(pT[0:64, hh_ * 128:(hh_ + 1) * 128], qtm[:, hh_ * 64:(hh_ + 1) * 64], identb[:])
                    return ins
                P.op("pe", fn, reads=[Bqtm, Bc], writes=[BpT])
                P.op("dve", lambda e: e.tensor_copy(out=qT[:].rearrange("p h t -> p (h t)"), in_=pT[0:64, 0:1024]), reads=[BpT], writes=[BqT])
                cut(32, XB, BXB)
                for hh_ in range(8):
                    hq = g * 8 + hh_

                    def fn(e, hh_=hh_, g=g, prv=prv, cur=cur):
                        e.matmul(pS[:, 0:128], kaT[prv][:, g, :], qT[:, hh_, :], start=True, stop=True)
                        return e.matmul(pS[:, 128:256], kaT[cur][:, g, :], qT[:, hh_, :], start=True, stop=True)
                    P.op("pe", fn, reads=[BkaT[0], BkaT[1], BqT], writes=[BpS])
                    P.op("dve", lambda e, hq=hq, dmt=dmt: e.scalar_tensor_tensor(out=sT[:], in0=dmt[:], scalar=-slopes[hq], in1=pS[:, 0:256],
                                                                                 op0=ALU.mult, op1=ALU.add), reads=[BpS, Bc], writes=[BsT])
                    P.op("act", lambda e: e.activation(out=eT[:], in_=sT[:], func=AF.Exp, scale=0.125), reads=[BsT], writes=[BeT])
                    cut(33, XB, BXB)

                    def fn(e, g=g, prv=prv, cur=cur):
                        e.matmul(pO[:, 0:66], eT[:, 0:128], vau[prv][:, g, 0:66], start=True, stop=False)
                        return e.matmul(pO[:, 0:66], eT[:, 128:256], vau[cur][:, g, 0:66], start=False, stop=True)
                    P.op("pe", fn, reads=[BeT, Bvau[0], Bvau[1]], writes=[BpO])
                    P.op("dve", lambda e, hq=hq: e.tensor_tensor(out=rden[:, 0:1], in0=pO[:, 64:65], in1=sinkexp[:, hq:hq + 1], op=ALU.add),
                         reads=[BpO, Bc], writes=[Brden])
                    P.op("dve", lambda e: e.reciprocal(out=rden[:, 1:2], in_=rden[:, 0:1]), writes=[Brden])
                    P.op("dve", lambda e, hq=hq: e.tensor_scalar(out=rotm[:, hq * 64:(hq + 1) * 64], in0=pO[:, 0:64], scalar1=rden[:, 1:2],
                                                                 scalar2=None, op0=ALU.mult), reads=[BpO, Brden], writes=[Brotm])
                    cut(34, XB, BXB)
            transposes_bf(rotm, Brotm, 2048, lambda c: aoT[:, c, :], BaoT)
            cut(3, XB, BXB)
            for hp in range(4):
                pp, Bpp = nextA()
                proj([(pp, Bpp)], [hl], wcols("w_in", (5 + hp) * 512), KC, key=("in", 5 + hp))
                P.op("act", lambda e, pp=pp: e.copy(out=rq[:], in_=pp[:]), reads=[Bpp], writes=[Brq])
                pp, Bpp = nextA()
                proj([(pp, Bpp)], [hl], wcols("w_in", (9 + hp) * 512), KC, key=("in", 9 + hp))
                P.op("act", lambda e, pp=pp: e.mul(out=rk[:], in_=pp[:], mul=1.0 / 16.0), reads=[Bpp], writes=[Brk])
                for h2 in range(2):
                    h = hp * 2 + h2
                    c0 = h2 * 256
                    pp, Bpp = nextA()
                    proj([(pp, Bpp)], [hl], wcols("w_in", (13 + h) * 512), KC, key=("in", 13 + h))
                    P.op("act", lambda e, pp=pp: e.copy(out=rv[:], in_=pp[:]), reads=[Bpp], writes=[Brv])
                    pp, Bpp = nextA()
                    proj([(pp, Bpp)], [hl], wcols("w_in", (21 + h) * 512), KC, key=("in", 21 + h))
                    P.op("act", lambda e, pp=pp: e.activation(out=rg[:], in_=pp[:], func=AF.Silu), reads=[Bpp], writes=[Brg])

                    def fn(e, c0=c0):
                        e.transpose(pT[:, 0:128], rq[:, c0:c0 + 128], identb[:])
                        e.transpose(pT[:, 128:256], rq[:, c0 + 128:c0 + 256], identb[:])
                        e.transpose(pT[:, 256:384], rk[:, c0:c0 + 128], identb[:])
                        return e.transpose(pT[:, 384:512], rk[:, c0 + 128:c0 + 256], identb[:])
                    P.op("pe", fn, reads=[Brq, Brk, Bc], writes=[BpT])
                    P.op("act", lambda e: e.copy(out=rqT[:].rearrange("p a b -> p (a b)"), in_=pT[:, 0:256]), reads=[BpT], writes=[BrqT])
                    for a_ in range(2):
                        P.op("dve", lambda e, h=h, a_=a_: e.tensor_tensor(out=rqdT[:, a_, :], in0=pT[:, a_ * 128:(a_ + 1) * 128],
                                                                         in1=qd[:, h, :], op=ALU.mult),
                             reads=[BpT, Bc], writes=[BrqdT])
                    P.op("act", lambda e: e.copy(out=rkT[:].rearrange("p a b -> p (a b)"), in_=pT[:, 256:512]), reads=[BpT], writes=[BrkT])
                    P.op("dve", lambda e, c0=c0, h=h: e.tensor_scalar(out=rkd[:], in0=rk[:, c0:c0 + 256], scalar1=kdec[:, h:h + 1], scalar2=None,
                                                                     op0=ALU.mult), reads=[Brk, Bc], writes=[Brkd])
                    P.op("act", lambda e, h=h: e.copy(out=Sbfh[:].rearrange("p a b -> p (a b)"), in_=S32[:, h].rearrange("p a b -> p (a b)")),
                         reads=[BS32[h]], writes=[BSbfh])

                    def fn(e):
                        e.matmul(pS[:, 0:128], rkT[:, 0, :], rqT[:, 0, :], start=True, stop=False)
                        return e.matmul(pS[:, 0:128], rkT[:, 1, :], rqT[:, 1, :], start=False, stop=True)
                    P.op("pe", fn, reads=[BrkT, BrqT], writes=[BpS])
                    P.op("dve", lambda e, h=h: e.tensor_tensor(out=adT[:], in0=pS[:, 0:128], in1=dmask[:, h, :], op=ALU.mult),
                         reads=[BpS, Bc], writes=[BadT])

                    def fn(e):
                        e.matmul(pO[:], adT[:], rv[:], start=True, stop=False)
                        e.matmul(pO[:], rqdT[:, 0, :], Sbfh[:, 0, :], start=False, stop=False)
                        return e.matmul(pO[:], rqdT[:, 1, :], Sbfh[:, 1, :], start=False, stop=True)
                    P.op("pe", fn, reads=[BadT, Brv, BrqdT, BSbfh], writes=[BpO])
                    for dc in range(2):
                        P.op("pe", lambda e, dc=dc: e.matmul(pU[dc][:], rkd[:, dc * 128:(dc + 1) * 128], rv[:], start=True, stop=True),
                             reads=[Brkd, Brv], writes=[BpU[dc]])
                        P.op("dve", lambda e, dc=dc, h=h: e.scalar_tensor_tensor(out=S32[:, h, dc, :], in0=S32[:, h, dc, :], scalar=gamC[h],
                                                                                in1=pU[dc][:], op0=ALU.mult, op1=ALU.add),
                             reads=[BpU[dc]], writes=[BS32[h]])
                    P.op("dve", lambda e: e.bn_stats(out=bnst[:], in_=pO[:]), reads=[BpO], writes=[Bbn])
                    P.op("dve", lambda e: e.bn_aggr(out=bnag[:], in_=bnst[:]), writes=[Bbn])
                    P.op("dve", lambda e: e.tensor_scalar(out=bnag[:, 1:2], in0=bnag[:, 1:2], scalar1=EPS, scalar2=None, op0=ALU.add), writes=[Bbn])
                    P.op("act", lambda e: e.activation(out=bnag[:, 1:2], in_=bnag[:, 1:2], func=AF.Sqrt), writes=[Bbn])
                    P.op("dve", lambda e: e.reciprocal(out=bnag[:, 1:2], in_=bnag[:, 1:2]), writes=[Bbn])
                    P.op("dve", lambda e: e.tensor_scalar(out=otmp[:], in0=pO[:], scalar1=bnag[:, 0:1], scalar2=bnag[:, 1:2],
                                                          op0=ALU.subtract, op1=ALU.mult), reads=[BpO, Bbn], writes=[Botmp])
                    P.op("dve", lambda e, h=h: e.tensor_tensor(out=rotm[:, h * 512:(h + 1) * 512], in0=otmp[:], in1=rg[:], op=ALU.mult),
                         reads=[Botmp, Brg], writes=[Brotm])
            transposes_bf(rotm, Brotm, D, lambda c: roT[:, c, :], BroT)
            cut(4, XB, BXB)
            for cb in range(8):
                cs = slice(cb * 512, (cb + 1) * 512)
                pp, Bpp = nextA()
                proj([(pp, Bpp)], [hl], wcols("w_in", (29 + cb) * 512), KC, key=("in", 29 + cb))
                P.op("act", lambda e, pp=pp: e.activation(out=sga[:], in_=pp[:], func=AF.Sigmoid), reads=[Bpp], writes=[Bsga])
                pp, Bpp = nextA()
                proj([(pp, Bpp)], [(aoT, BaoT, slice(0, 128))], W["w_ao"][:, cs], 16, key=("ao", cb))
                P.op("dve", lambda e, pp=pp, cs=cs: e.tensor_tensor(out=XB[:, cs], in0=sga[:], in1=pp[:], op=ALU.mult), reads=[Bpp, Bsga], writes=[BXB])
                pp, Bpp = nextA()
                proj([(pp, Bpp)], [hl], wcols("w_in", (37 + cb) * 512), KC, key=("in", 37 + cb))
                P.op("act", lambda e, pp=pp: e.activation(out=sgb[:], in_=pp[:], func=AF.Sigmoid), reads=[Bpp], writes=[Bsgb])
                pp, Bpp = nextA()
                proj([(pp, Bpp)], [(roT, BroT, slice(0, 128))], W["w_ro"][:, cs], KC, key=("ro", cb))
                P.op("dve", lambda e, pp=pp: e.tensor_tensor(out=sgb[:], in0=sgb[:], in1=pp[:], op=ALU.mult), reads=[Bpp], writes=[Bsgb])
                P.op("dve", lambda e, cs=cs: e.tensor_tensor(out=rotm[:, cs], in0=sgb[:], in1=XB[:, cs], op=ALU.add), reads=[Bsgb, BXB], writes=[Brotm])
            transposes_bf(rotm, Brotm, D, lambda c: roT[:, c, :], BroT)
            cut(5, XB, BXB)
            for cb in range(8):
                cs = slice(cb * 512, (cb + 1) * 512)
                pp, Bpp = nextA()
                proj([(pp, Bpp)], [(roT, BroT, slice(0, 128))], W["w_o"][:, cs], KC, key=("o", cb))
                P.op("dve", lambda e, pp=pp, cs=cs: e.tensor_tensor(out=sga[:], in0=pp[:], in1=gbc[:, cs], op=ALU.mult), reads=[Bpp, Bgbc], writes=[Bsga])
                P.op("dve", lambda e, cs=cs: e.tensor_tensor(out=XA[:, cs], in0=XA[:, cs], in1=sga[:], op=ALU.add), reads=[Bsga], writes=[BXA])
            if not full:
                P.op("sp", lambda e, tg=tg: e.dma_start(out=out_d[tg * 128:(tg + 1) * 128, :], in_=XA[:]), reads=[BXA], writes=[Bx1d], dma="out")
                if tg == 0 and nseq > 1:
                    P.barrier()
                continue
            P.op("sp", lambda e, tg=tg: e.dma_start(out=x1_d[tg * 128:(tg + 1) * 128, :], in_=XA[:]), reads=[BXA], writes=[Bx1d], dma="x1o")
            rms_to_T(XA, BXA, XB, BXB, G2, SH2, hT, BhT, router=True)
            P.op("sp", lambda e, tg=tg: e.dma_start(out=h2T_d[:, :, tg * 128:(tg + 1) * 128], in_=hT[:]), reads=[BhT], writes=[Bh2d], dma="h2o")
            sc, ch, t1, t2, cm, rw = rt["sc"], rt["ch"], rt["t1"], rt["t2"], rt["cm"], rt["rw"]
            m1, m2, gs, top, gm, mx = rt8["m1"], rt8["m2"], rt8["gs"], rt8["top"], rt8["gm"], rt8["mx"]
            R = dict(reads=[Bc], writes=[Brt])
            P.op("act", lambda e: e.activation(out=sc[:], in_=pS[:, 0:64], func=AF.Sigmoid), reads=[BpS], writes=[Brt])
            P.op("dve", lambda e: e.tensor_tensor(out=ch[:], in0=sc[:], in1=brbc[:], op=ALU.add), **R)
            ch3 = ch[:].rearrange("p (g k) -> p g k", g=8)
            P.op("dve", lambda e: e.tensor_reduce(out=m1[:], in_=ch3, axis=AX.X, op=ALU.max), **R)
            P.op("dve", lambda e: e.tensor_tensor(out=t1[:].rearrange("p (g k) -> p g k", g=8), in0=ch3,
                                                  in1=m1[:].unsqueeze(2).to_broadcast([128, 8, 8]), op=ALU.is_equal), **R)
            P.op("dve", lambda e: e.scalar_tensor_tensor(out=t2[:], in0=t1[:], scalar=-1.0e9, in1=ch[:], op0=ALU.mult, op1=ALU.add), **R)
            P.op("dve", lambda e: e.tensor_reduce(out=m2[:], in_=t2[:].rearrange("p (g k) -> p g k", g=8), axis=AX.X, op=ALU.max), **R)
            P.op("dve", lambda e: e.tensor_tensor(out=gs[:], in0=m1[:], in1=m2[:], op=ALU.add), **R)
            P.op("dve", lambda e: e.max(out=top[:], in_=gs[:]), **R)
            P.op("dve", lambda e: e.tensor_scalar(out=gm[:], in0=gs[:], scalar1=top[:, 3:4], scalar2=None, op0=ALU.is_ge), **R)
            P.op("dve", lambda e: e.tensor_scalar(out=gm[:], in0=gm[:], scalar1=-1.0, scalar2=1.0e9, op0=ALU.add, op1=ALU.mult), **R)
            P.op("dve", lambda e: e.tensor_tensor(out=cm[:].rearrange("p (g k) -> p g k", g=8), in0=ch3,
                                                  in1=gm[:].unsqueeze(2).to_broadcast([128, 8, 8]), op=ALU.add), **R)
            P.op("dve", lambda e: e.max(out=mx[:], in_=cm[:]), **R)
            P.op("dve", lambda e: e.tensor_scalar(out=t1[:], in0=cm[:], scalar1=mx[:, 7:8], scalar2=None, op0=ALU.is_ge), **R)
            P.op("dve", lambda e: e.tensor_tensor(out=t2[:], in0=t1[:], in1=sc[:], op=ALU.mult), **R)
            P.op("dve", lambda e: e.tensor_reduce(out=m1[:, 0:1], in_=t2[:], axis=AX.X, op=ALU.add), **R)
            P.op("dve", lambda e: e.reciprocal(out=m1[:, 1:2], in_=m1[:, 0:1]), **R)
            P.op("dve", lambda e: e.tensor_scalar(out=rw[:], in0=t2[:], scalar1=m1[:, 1:2], scalar2=2.5, op0=ALU.mult, op1=ALU.mult), **R)
            P.op("sp", lambda e, tg=tg: e.dma_start(out=rw_d[:, tg, :], in_=rw[:]), reads=[Brt], writes=[Brwd], dma="rwo")
            if tg == 0 and nseq > 1:
                P.barrier()

        if full:
            P.barrier()
            S32v = S32[:].rearrange("p h a b -> p (h a b)")
            X1L = S32v[:, 0:D]; GF = S32v[:, D:2 * D]
            BX1L = Buf("X1L"); BGF = Buf("GF")
            yacc = [XA, XB]; Byacc = [Buf("yacc0"), Buf("yacc1")]
            h2s = [hT, roT]; Bh2s = [Buf("h2s0"), Buf("h2s1")]
            hh = sb("hh", [128, 512], BF16); Bhh = Buf("hh")
            hhT = [sb("hhT%d" % j, [128, 4, 128], BF16) for j in range(2)]; BhhT = [Buf("hhT%d" % j) for j in range(2)]
            RW2 = sb("RW2", [128, 2, 64]); BRW2 = Buf("RW2")
            ones1 = sb("ones1", [128, 1]); Bones = Buf("ones1")
            P.op("dve", lambda e: e.memset(ones1[:], 1.0), writes=[Bones])
            Bgbc2 = Buf("gbc2")
            P.op("sp", lambda e: e.dma_start(out=gbc[:], in_=mod_d[:, 5 * D:6 * D].partition_broadcast(128)), writes=[Bgbc2], dma="gbc")
            P.op("sp", lambda e: e.dma_start(out=GF, in_=b_ada_in[:, 5 * D:6 * D].partition_broadcast(128)), writes=[BGF], dma="gf")
            P.op("dve", lambda e: e.tensor_tensor(out=gbc[:], in0=gbc[:], in1=GF, op=ALU.add), reads=[BGF], writes=[Bgbc2])
            P.op("sp", lambda e: e.dma_start(out=GF, in_=g_fin_in.partition_broadcast(128)), reads=[Bgbc2], writes=[BGF], dma="gf")
            gsrc = [(pA[0], BpA[0]), (pA[1], BpA[1])]
            usrc = [(pS, BpS), (pO, BpO)]
            for ps_ in range(nseq // 2):
                for j in range(2):
                    tg = ps_ * 2 + j
                    P.op("sp", lambda e, j=j, tg=tg: e.dma_start(out=h2s[j][:], in_=h2T_d[:, :, tg * 128:(tg + 1) * 128]), writes=[Bh2s[j]], dma="mh%d" % j)
                    P.op("dve", lambda e, j=j: e.memset(yacc[j][:], 0.0), writes=[Byacc[j]])
                P.op("sp", lambda e, ps_=ps_: e.dma_start(out=RW2[:], in_=rw_d[:, ps_ * 2:ps_ * 2 + 2, :]), writes=[BRW2], dma="rw2")
                lhs = [(h2s[0], Bh2s[0], slice(0, 128)), (h2s[1], Bh2s[1], slice(0, 128))]
                for ex in range(65):
                    if ex < 64:
                        sg_, su_, sd_ = W["w_gate"][ex], W["w_up"][ex], W["w_down"][ex]
                    else:
                        sg_, su_, sd_ = W["w_sg"], W["w_su"], W["w_sd"]
                    proj(gsrc, lhs, sg_, KC, key=("g", ex))
                    proj(usrc, lhs, su_, KC, key=("u", ex))
                    for j in range(2):
                        pg, Bpg = gsrc[j]
                        pu, Bpu = usrc[j]
                        P.op("act", lambda e, pg=pg: e.activation(out=sga[:], in_=pg[:], func=AF.Silu), reads=[Bpg], writes=[Bsga])
                        wsc = RW2[:, j, ex:ex + 1] if ex < 64 else ones1[:, 0:1]
                        P.op("dve", lambda e, pu=pu, wsc=wsc: e.scalar_tensor_tensor(out=hh[:], in0=sga[:], scalar=wsc, in1=pu[:], op0=ALU.mult, op1=ALU.mult),
                             reads=[Bsga, Bpu, BRW2, Bones], writes=[Bhh])

                        def fn(e):
                            ins = None
                            for m in range(4):
                                ins = e.transpose(pT[:, m * 128:(m + 1) * 128], hh[:, m * 128:(m + 1) * 128], identb[:])
                            return ins
                        P.op("pe", fn, reads=[Bhh, Bc], writes=[BpT])
                        P.op("act", lambda e, j=j: e.copy(out=hhT[j][:].rearrange("p a b -> p (a b)"), in_=pT[:, 0:512]), reads=[BpT], writes=[BhhT[j]])
                    for cq in range(4):
                        wdt, Bwdt = load_raw(("d", ex, cq), lambda e, i, sd_=sd_, cq=cq: e.dma_start(
                            out=wb[i][:].rearrange("p (m c) n -> p m (c n)", m=4),
                            in_=sd_[:, cq * 1024:(cq + 1) * 1024].rearrange("(m p) n -> p m n", p=128)))
                        for j in range(2):
                            for c2 in range(2):
                                py, Bpy = pU[c2], BpU[c2]
                                cs = slice(cq * 1024 + c2 * 512, cq * 1024 + (c2 + 1) * 512)

                                def fn(e, py=py, c2=c2, wdt=wdt, j=j):
                                    ins = None
                                    for m in range(4):
                                        ins = e.matmul(py[:], hhT[j][:, m, :], wdt[:, m * 2 + c2, :], start=(m == 0), stop=(m == 3))
                                    return ins
                                P.op("pe", fn, reads=[BhhT[j], Bwdt], writes=[Bpy])
                                P.op("dve", lambda e, py=py, cs=cs, j=j: e.tensor_tensor(out=yacc[j][:, cs], in0=yacc[j][:, cs], in1=py[:], op=ALU.add),
                                     reads=[Bpy], writes=[Byacc[j]])
                for j in range(2):
                    tg = ps_ * 2 + j
                    P.op("sp", lambda e, tg=tg: e.dma_start(out=X1L, in_=x1_d[tg * 128:(tg + 1) * 128, :]), writes=[BX1L], dma="x1i")
                    P.op("dve", lambda e, j=j: e.tensor_tensor(out=yacc[j][:], in0=yacc[j][:], in1=gbc[:], op=ALU.mult), reads=[Bgbc2], writes=[Byacc[j]])
                    P.op("dve", lambda e, j=j: e.tensor_tensor(out=yacc[j][:], in0=yacc[j][:], in1=X1L, op=ALU.add), reads=[BX1L], writes=[Byacc[j]])
                    P.op("act", lambda e, j=j: e.activation(out=rotm[:], in_=yacc[j][:], func=AF.Square, accum_out=st1[:, 0:1]),
                         reads=[Byacc[j]], writes=[Brotm, Bst])
                    P.op("dve", lambda e: e.tensor_scalar(out=st1[:, 1:2], in0=st1[:, 0:1], scalar1=1.0 / D, scalar2=EPS, op0=ALU.mult, op1=ALU.add), writes=[Bst])
                    P.op("act", lambda e: e.activation(out=st1[:, 3:4], in_=st1[:, 1:2], func=AF.Sqrt), writes=[Bst])
                    P.op("dve", lambda e: e.reciprocal(out=st1[:, 2:3], in_=st1[:, 3:4]), writes=[Bst])
                    P.op("dve", lambda e, j=j: e.scalar_tensor_tensor(out=X1L, in0=yacc[j][:], scalar=st1[:, 2:3], in1=GF, op0=ALU.mult, op1=ALU.mult),
                         reads=[Byacc[j], Bst, BGF], writes=[BX1L])
                    P.op("sp", lambda e, tg=tg: e.dma_start(out=out_d[tg * 128:(tg + 1) * 128, :], in_=X1L), reads=[BX1L], dma="out")
                if ps_ == 0 and nseq > 2:
                    P.barrier()
    try:
        _body()
    except _Cut:
        pass
    P.barrier()
    P.emit()
    es.close()
    return nc


def _tables():
    q = np.arange(128)[None, :]
    k = np.arange(128)[:, None]
    dcur = (q - k).astype(np.float32)
    dprev = (q + 128 - k).astype(np.float32)
    cur = np.where(dcur >= 0, 8.0 * dcur, BIG).astype(np.float32)
    prev = np.where(dprev < 128, 8.0 * dprev, BIG).astype(np.float32)
    dmg = np.concatenate([prev, cur], axis=1)
    dm0 = np.concatenate([np.full_like(prev, BIG), cur], axis=1)
    gam = 1.0 - 2.0 ** (-5.0 - np.arange(8, dtype=np.float64))
    i = np.arange(128)
    dmask = np.zeros((128, 8, 128), np.float32)
    qd = np.zeros((128, 8, 128), np.float32)
    kdec = np.zeros((128, 8), np.float32)
    for h in range(8):
        rel = i[None, :] - i[:, None]
        dmask[:, h, :] = np.where(rel >= 0, gam[h] ** np.maximum(rel, 0), 0.0)
        qd[:, h, :] = (gam[h] ** (i + 1.0))[None, :]
        kdec[:, h] = gam[h] ** (127.0 - i)
    return dmg, dm0, dmask.reshape(128, -1), qd.reshape(128, -1), kdec


def make_in_map(inp, xs, cvec, full=True):
    dmg, dm0, dmask, qd, kdec = _tables()
    f32 = lambda a: np.ascontiguousarray(np.asarray(a, np.float32))
    fm = lambda v, n=KC: np.ascontiguousarray(np.asarray(v, np.float32).reshape(n, 128).T)
    m = {
        "x_seq": f32(xs), "c_fm": fm(cvec), "dm0": dm0, "dmg": dmg, "dmask": dmask, "qd": qd, "kdec": kdec,
        "identb": np.eye(128, dtype=np.float32).astype(ml_dtypes.bfloat16), "identf": np.eye(128, dtype=np.float32),
        "b_ada": f32(inp["b_ada"]).reshape(1, -1), "b_ada_fm": fm(inp["b_ada"], 6 * KC),
        "g_mix_fm": fm(inp["g_norm_mix"]), "g_ffn_fm": fm(inp["g_norm_ffn"]),
        "g_fin": f32(inp["g_norm_final"]).reshape(1, -1), "sinks": f32(inp["attn_sinks"]).reshape(1, -1),
        "b_router": f32(inp["b_router"]).reshape(1, -1),
        "w_ada": f32(inp["w_ada"][0]), "w_in": f32(inp["w_in"][0]), "w_ao": f32(inp["w_attn_out"][0]),
        "w_ro": f32(inp["w_ret_out"][0]), "w_o": f32(inp["w_o"][0]), "w_router": f32(inp["w_router"][0]),
    }
    if full:
        m.update({"w_gate": f32(inp["w_gate"][0]), "w_up": f32(inp["w_up"][0]), "w_down": f32(inp["w_down"][0]),
                  "w_sg": f32(inp["w_sh_gate"][0]), "w_su": f32(inp["w_sh_up"][0]), "w_sd": f32(inp["w_sh_down"][0])})
    return m


def kernel(**inp):
    x = np.asarray(inp["x"], np.float32)
    c = np.asarray(inp["c"], np.float32)
    nc = build_program(32, "full")
    in_maps = [make_in_map(inp, x[b], c[b]) for b in range(NCORE)]
    res = run_bass_kernel_spmd(nc, in_maps, core_ids=list(range(NCORE)))
    return np.stack([res.results[b]["out"] for b in range(NCORE)], axis=0).astype(np.float32)
```

```python
import numpy as np
from contextlib import ExitStack
import ml_dtypes
import concourse.bass as bass
import concourse.mybir as mybir
from concourse.bass_utils import run_bass_kernel_spmd

F32 = mybir.dt.float32
BF16 = mybir.dt.bfloat16
AF = mybir.ActivationFunctionType
ALU = mybir.AluOpType
AX = mybir.AxisListType

D = 4096
KC = 32
NCORE = 4
N_IN = 23040
EPS = 1e-6
BIG = 1.0e6
KG = 8
NWB = 4


class Buf:
    __slots__ = ("name", "lw", "rd")

    def __init__(self, name):
        self.name = name
        self.lw = None
        self.rd = {}


class Prog:
    ENG = ("pe", "act", "dve", "pool", "sp")

    def __init__(self, nc, es):
        self.nc = nc
        self.es = es
        self.streams = {e: [] for e in self.ENG}
        self.sems = {}
        self.cnt = {}
        self.known = {e: {} for e in self.ENG}
        for e in self.ENG:
            self._sem("E_" + e)

    def _sem(self, key):
        if key not in self.sems:
            self.sems[key] = self.es.enter_context(self.nc.semaphore(key))
            self.cnt[key] = 0
        return self.sems[key]

    def op(self, eng, fn, reads=(), writes=(), dma=None):
        deps = []
        for b in reads:
            if b.lw:
                deps.append(b.lw)
        for b in writes:
            if b.lw:
                deps.append(b.lw)
            deps.extend(b.rd.items())
        if dma is None:
            key, inc = "E_" + eng, 1
        else:
            key, inc = "D_" + dma, 16
            self._sem(key)
        waits = {}
        kn = self.known[eng]
        for k, v in deps:
            if k == "E_pe" and eng == "pe":
                continue
            if kn.get(k, 0) >= v:
                continue
            if waits.get(k, 0) < v:
                waits[k] = v
        for k, v in waits.items():
            kn[k] = v
        self.cnt[key] += inc
        done = (key, self.cnt[key])
        self.streams[eng].append((fn, list(waits.items()), key, inc))
        for b in writes:
            b.lw = done
            b.rd = {}
        for b in reads:
            if b not in writes:
                if b.rd.get(key, 0) < done[1]:
                    b.rd[key] = done[1]
        return done

    def barrier(self):
        allc = [(k, v) for k, v in self.cnt.items() if v > 0]
        for e in self.ENG:
            w = [(k, v) for k, v in allc if self.known[e].get(k, 0) < v and not (k == "E_" + e)]
            for k, v in w:
                self.known[e][k] = v
            self.streams[e].append((None, w, None, 0))

    def emit(self):
        nc = self.nc
        sems = self.sems
        streams = self.streams
        with nc.Block() as block:
            def mk(e):
                def body(eng):
                    for fn, waits, key, inc in streams[e]:
                        for k, v in waits:
                            eng.wait_ge(sems[k], v)
                        if fn is not None:
                            fn(eng).then_inc(sems[key], inc)
                return body
            block.tensor(mk("pe"))
            block.scalar(mk("act"))
            block.vector(mk("dve"))
            block.gpsimd(mk("pool"))
            block.sync(mk("sp"))


class _Cut(Exception):
    pass


def build_program(nseq=32, stage="full", cutn=0, main0=0):
    nc = bass.Bass("TRN2", target_bir_lowering=False)
    es = ExitStack()
    P = Prog(nc, es)
    full = stage == "full"
    nmain = nseq - main0

    def din(name, shape, dt=F32):
        return nc.dram_tensor(name, list(shape), dt, kind="ExternalInput").ap()

    def dint(name, shape, dt=F32):
        return nc.dram_tensor(name, list(shape), dt, kind="Internal").ap()

    def sb(name, shape, dt=F32):
        return es.enter_context(nc.sbuf_tensor("s_" + name, list(shape), dt))

    def ps(name, shape, dt=F32):
        return es.enter_context(nc.psum_tensor("p_" + name, list(shape), dt))

    x_seq = din("x_seq", [nseq * 128, D])
    c_fm = din("c_fm", [128, KC])
    dm0_in = din("dm0", [128, 256]); dmg_in = din("dmg", [128, 256]); dmx_in = din("dmx", [128, 256]); flag_in = din("flag", [128, 1])
    dmask_in = din("dmask", [128, 8 * 128]); qd_in = din("qd", [128, 8 * 128]); kdec_in = din("kdec", [128, 8])
    identb_in = din("identb", [128, 128], BF16); identf_in = din("identf", [128, 128])
    b_ada_in = din("b_ada", [1, 6 * D]); b_ada_fm = din("b_ada_fm", [128, 6 * KC])
    g_mix_fm = din("g_mix_fm", [128, KC]); g_ffn_fm = din("g_ffn_fm", [128, KC])
    g_fin_in = din("g_fin", [1, D]); sinks_in = din("sinks", [1, 32]); b_router_in = din("b_router", [1, 64])
    W = {"w_ada": din("w_ada", [D, 6 * D]), "w_in": din("w_in", [D, N_IN]), "w_ao": din("w_ao", [2048, D]),
         "w_ro": din("w_ro", [D, D]), "w_o": din("w_o", [D, D]), "w_router": din("w_router", [D, 64])}
    if full:
        W.update({"w_gate": din("w_gate", [64, D, 512]), "w_up": din("w_up", [64, D, 512]), "w_down": din("w_down", [64, 512, D]),
                  "w_sg": din("w_sg", [D, 512]), "w_su": din("w_su", [D, 512]), "w_sd": din("w_sd", [512, D])})
    mod_d = dint("mod_d", [1, 6 * D])
    x1_d = dint("x1_d", [nmain * 128, D])
    h2T_d = dint("h2T_d", [128, KC, nmain * 128], BF16)
    rw_d = dint("rw_d", [128, nmain, 64])
    out_d = nc.dram_tensor("out", [nmain * 128, D], F32, kind="ExternalOutput").ap()

    identb = sb("identb", [128, 128], BF16); identf = sb("identf", [128, 128])
    dm0 = sb("dm0", [128, 256]); dmg = sb("dmg", [128, 256]); dmx = sb("dmx", [128, 256]); flag = sb("flag", [128, 1])
    dmask = sb("dmask", [128, 8, 128]); qd = sb("qd", [128, 8, 128]); kdec = sb("kdec", [128, 8])
    cfm = sb("cfm", [128, KC]); cbf = sb("cbf", [128, KC], BF16)
    gmix = sb("gmix", [128, KC]); gffn = sb("gffn", [128, KC])
    modfm = sb("modfm", [128, 6 * KC]); bfm = sb("bfm", [128, 6 * KC])
    G1 = sb("G1", [128, KC]); G2 = sb("G2", [128, KC]); G1f = sb("G1f", [128, KC]); SH1f = sb("SH1f", [128, KC])
    sinkexp = sb("sinkexp", [128, 32]); brbc = sb("brbc", [128, 64])
    wr_sb = sb("wr_sb", [128, KC, 64])
    Bc = Buf("consts"); Bmod = Buf("mod")
    for t, src in [(identb, identb_in), (identf, identf_in), (dm0, dm0_in), (dmg, dmg_in), (dmx, dmx_in), (flag, flag_in), (dmask, dmask_in), (qd, qd_in),
                   (kdec, kdec_in), (cfm, c_fm), (gmix, g_mix_fm), (gffn, g_ffn_fm), (bfm, b_ada_fm)]:
        tv = t[:].rearrange("p a b -> p (a b)") if len(t.shape) == 3 else t[:]
        P.op("sp", lambda e, tv=tv, src=src: e.dma_start(out=tv, in_=src), writes=[Bc], dma="c")
    P.op("sp", lambda e: e.dma_start(out=sinkexp[:], in_=sinks_in.partition_broadcast(128)), writes=[Bc], dma="c")
    P.op("sp", lambda e: e.dma_start(out=brbc[:], in_=b_router_in.partition_broadcast(128)), writes=[Bc], dma="c")
    P.op("sp", lambda e: e.dma_start(out=wr_sb[:], in_=W["w_router"].rearrange("(kc p) n -> p kc n", p=128)), writes=[Bc], dma="c")
    P.op("act", lambda e: e.activation(out=sinkexp[:], in_=sinkexp[:], func=AF.Exp), reads=[Bc], writes=[Bc])
    P.op("act", lambda e: e.activation(out=cbf[:], in_=cfm[:], func=AF.Silu), reads=[Bc], writes=[Bmod])

    pA = [ps("pA%d" % i, [128, 512]) for i in range(2)]; BpA = [Buf("pA%d" % i) for i in range(2)]
    pT = ps("pT", [128, 1024], BF16); BpT = Buf("pT")
    pF = ps("pF", [128, 512]); BpF = Buf("pF")
    pS = ps("pS", [128, 512]); BpS = Buf("pS")
    pO = ps("pO", [128, 512]); BpO = Buf("pO")
    pU = [ps("pU%d" % i, [128, 512]) for i in range(2)]; BpU = [Buf("pU%d" % i) for i in range(2)]
    pa_i = [0]

    def nextA():
        i = pa_i[0]
        pa_i[0] ^= 1
        return pA[i], BpA[i]

    wb = [sb("wb%d" % i, [128, KG, 512], BF16) for i in range(NWB)]
    Bwb = [Buf("wb%d" % i) for i in range(NWB)]
    wb_i = [0]

    SCR_N = 240
    scr_pools = []
    scr_map = {}

    def scratch_for(key):
        if key not in scr_map:
            idx = len(scr_map)
            if idx % SCR_N == 0:
                scr_pools.append(dint("scr%d" % (idx // SCR_N), [SCR_N, 128, KG * 512], BF16))
            scr_map[key] = (scr_pools[-1][idx % SCR_N], Buf("scr%d" % idx), False)
        return scr_map[key]

    def load_raw(key, first_fn):
        i = wb_i[0]
        wb_i[0] = (i + 1) % NWB
        flat = wb[i][:].rearrange("p a b -> p (a b)")
        if key is None:
            P.op("pool", lambda e, i=i: first_fn(e, i), writes=[Bwb[i]], dma="wb%d" % i)
            return wb[i], Bwb[i]
        ap, Bs, seen = scratch_for(key)
        if not seen:
            P.op("pool", lambda e, i=i: first_fn(e, i), writes=[Bwb[i]], dma="wb%d" % i)
            P.op("sp", lambda e, ap=ap, flat=flat: e.dma_start(out=ap, in_=flat), reads=[Bwb[i]], writes=[Bs], dma="scrw%d" % i)
            scr_map[key] = (ap, Bs, True)
        else:
            P.op("pool", lambda e, ap=ap, flat=flat: e.dma_start(out=flat, in_=ap), reads=[Bs], writes=[Bwb[i]], dma="wb%d" % i)
        return wb[i], Bwb[i]

    def load_tile(view, nk=KG, ncol=512, key=None):
        assert key is None or (nk == KG and ncol == 512)
        v = view.rearrange("(kc p) n -> p kc n", p=128)
        return load_raw(key, lambda e, i: e.dma_start(out=wb[i][:, 0:nk, 0:ncol], in_=v))

    def proj(outs, lhs_list, src, nk, ncol=512, key=None):
        ng = nk // KG
        for g in range(ng):
            w, Bwt = load_tile(src[g * KG * 128:(g + 1) * KG * 128, :], KG, ncol, key=None if key is None else key + (g,))
            for (outp, Bout), (actT, Bact, cs) in zip(outs, lhs_list):
                def fn(e, g=g, w=w, outp=outp, actT=actT, cs=cs):
                    ins = None
                    for k in range(KG):
                        kc = g * KG + k
                        ins = e.matmul(outp[:, 0:ncol], actT[:, kc, cs], w[:, k, 0:ncol], start=(kc == 0), stop=(kc == nk - 1))
                    return ins
                P.op("pe", fn, reads=[Bact, Bwt], writes=[Bout])

    XA = sb("XA", [128, D]); BXA = Buf("XA")
    XB = sb("XB", [128, D]); BXB = Buf("XB")
    gbc = sb("gbc", [128, D]); Bgbc = Buf("gbc")
    S32 = sb("S32", [128, 8, 2, 512]); BS32 = [Buf("S32_%d" % h) for h in range(8)]
    hT = sb("hT", [128, KC, 128], BF16); BhT = Buf("hT")
    aoT = sb("aoT", [128, 16, 128], BF16); BaoT = Buf("aoT")
    roT = sb("roT", [128, KC, 128], BF16); BroT = Buf("roT")
    rotm = sb("rotm", [128, D], BF16); Brotm = Buf("rotm")
    st1 = sb("st1", [128, 8]); Bst = Buf("st1")

    mrow = [sb("mrow%d" % i, [1, 512]) for i in range(2)]; Bmrow = [Buf("mrow%d" % i) for i in range(2)]
    Bmd = Buf("mod_d")
    for cb in range(48):
        pp, Bpp = nextA()
        src = W["w_ada"][:, cb * 512:(cb + 1) * 512]
        for g in range(KC // KG):
            w, Bwt = load_tile(src[g * KG * 128:(g + 1) * KG * 128, :])

            def fn(e, g=g, w=w, pp=pp):
                ins = None
                for k in range(KG):
                    kc = g * KG + k
                    ins = e.matmul(pp[0:1, :], cbf[:, kc:kc + 1], w[:, k, :], start=(kc == 0), stop=(kc == KC - 1))
                return ins
            P.op("pe", fn, reads=[Bmod, Bwt], writes=[Bpp])
        mi = cb % 2
        P.op("dve", lambda e, pp=pp, mi=mi: e.tensor_copy(out=mrow[mi][:], in_=pp[0:1, :]), reads=[Bpp], writes=[Bmrow[mi]])
        P.op("sp", lambda e, mi=mi, cb=cb: e.dma_start(out=mod_d[:, cb * 512:(cb + 1) * 512], in_=mrow[mi][:]),
             reads=[Bmrow[mi]], writes=[Bmd], dma="modo")

        def fn(e, mi=mi):
            ins = None
            for j in range(4):
                ins = e.transpose(pF[:, j:j + 1], mrow[mi][0:1, j * 128:(j + 1) * 128], identf[0:1, 0:1])
            return ins
        P.op("pe", fn, reads=[Bmrow[mi], Bc], writes=[BpF])
        P.op("dve", lambda e, cb=cb: e.tensor_copy(out=modfm[:, cb * 4:(cb + 1) * 4], in_=pF[:, 0:4]), reads=[BpF], writes=[Bmod])
    P.op("dve", lambda e: e.tensor_tensor(out=modfm[:], in0=modfm[:], in1=bfm[:], op=ALU.add), reads=[Bc], writes=[Bmod])
    P.op("dve", lambda e: e.scalar_tensor_tensor(out=G1[:], in0=modfm[:, 32:64], scalar=1.0, in1=gmix[:], op0=ALU.add, op1=ALU.mult),
         reads=[Bc], writes=[Bmod])
    P.op("dve", lambda e: e.scalar_tensor_tensor(out=G2[:], in0=modfm[:, 128:160], scalar=1.0, in1=gffn[:], op0=ALU.add, op1=ALU.mult),
         reads=[Bc], writes=[Bmod])
    SH1 = modfm[:, 0:32]
    SH2 = modfm[:, 96:128]
    P.op("dve", lambda e: e.tensor_scalar(out=G1f[:], in0=G1[:], scalar1=flag[:, 0:1], scalar2=None, op0=ALU.mult), reads=[Bc], writes=[Bmod])
    P.op("dve", lambda e: e.tensor_scalar(out=SH1f[:], in0=modfm[:, 0:32], scalar1=flag[:, 0:1], scalar2=None, op0=ALU.mult), reads=[Bc], writes=[Bmod])
    P.op("sp", lambda e: e.dma_start(out=gbc[:], in_=mod_d[:, 2 * D:3 * D].partition_broadcast(128)), reads=[Bmd], writes=[Bgbc], dma="gbc")
    P.op("sp", lambda e: e.dma_start(out=XB[:], in_=b_ada_in[:, 2 * D:3 * D].partition_broadcast(128)), writes=[BXB], dma="xb")
    P.op("dve", lambda e: e.tensor_tensor(out=gbc[:], in0=gbc[:], in1=XB[:], op=ALU.add), reads=[BXB], writes=[Bgbc])

    kaT = [sb("kaT%d" % i, [64, 4, 128], BF16) for i in range(2)]; BkaT = [Buf("kaT%d" % i) for i in range(2)]
    vau = [sb("vau%d" % i, [128, 4, 72], BF16) for i in range(2)]; Bvau = [Buf("vau%d" % i) for i in range(2)]
    kvtm = sb("kvtm", [128, 256], BF16); Bkvtm = Buf("kvtm")
    qtm = sb("qtm", [128, 512], BF16); Bqtm = Buf("qtm")
    qT = sb("qT", [64, 8, 128], BF16); BqT = Buf("qT")
    sT = sb("sT", [128, 256]); BsT = Buf("sT")
    eT = sb("eT", [128, 256], BF16); BeT = Buf("eT")
    rden = sb("rden", [128, 2]); Brden = Buf("rden")
    rq = sb("rq", [128, 512], BF16); Brq = Buf("rq")
    rk = sb("rk", [128, 512], BF16); Brk = Buf("rk")
    rv = sb("rv", [128, 512], BF16); Brv = Buf("rv")
    rg = sb("rg", [128, 512], BF16); Brg = Buf("rg")
    rqT = sb("rqT", [128, 2, 128], BF16); BrqT = Buf("rqT")
    rqdT = sb("rqdT", [128, 2, 128], BF16); BrqdT = Buf("rqdT")
    rkT = sb("rkT", [128, 2, 128], BF16); BrkT = Buf("rkT")
    rkd = sb("rkd", [128, 256], BF16); Brkd = Buf("rkd")
    adT = sb("adT", [128, 128], BF16); BadT = Buf("adT")
    Sbfh = sb("Sbfh", [128, 2, 512], BF16); BSbfh = Buf("Sbfh")
    bnst = sb("bnst", [128, 6]); bnag = sb("bnag", [128, 2]); Bbn = Buf("bn")
    otmp = sb("otmp", [128, 512]); Botmp = Buf("otmp")
    sga = sb("sga", [128, 512]); Bsga = Buf("sga")
    sgb = sb("sgb", [128, 512]); Bsgb = Buf("sgb")
    h32g = sb("h32g", [128, 4, 128]); Bh32g = Buf("h32g")
    rt = {n: sb("rt_" + n, [128, 64]) for n in ("sc", "ch", "t1", "t2", "cm", "rw")}
    rt8 = {n: sb("rt8_" + n, [128, 8]) for n in ("m1", "m2", "gs", "top", "gm", "mx")}
    Brt = Buf("rt")
    Bx1d = Buf("x1_d"); Bh2d = Buf("h2T_d"); Brwd = Buf("rw_d")

    for h in range(8):
        P.op("dve", lambda e, h=h: e.memset(S32[:, h].rearrange("p a b -> p (a b)"), 0.0), writes=[BS32[h]])
    for i in range(2):
        P.op("dve", lambda e, i=i: e.memset(vau[i][:].rearrange("p a b -> p (a b)"), 0.0), writes=[Bvau[i]])
        P.op("dve", lambda e, i=i: e.memset(vau[i][:, :, 64:65], 1.0), writes=[Bvau[i]])
        P.op("dve", lambda e, i=i: e.memset(kaT[i][:].rearrange("p a b -> p (a b)"), 0.0), writes=[BkaT[i]])

    slopes = [float(2.0 ** (-8.0 * (h + 1) / 32)) for h in range(32)]
    gamC = [float((1.0 - 2.0 ** (-5.0 - h)) ** 128) for h in range(8)]

    def transposes_bf(src, Bsrc, ncol, dst_fn, Bdst):
        nch = ncol // 128
        for g0 in range(0, nch, 8):
            n = min(8, nch - g0)

            def fn(e, g0=g0, n=n):
                ins = None
                for j in range(n):
                    ins = e.transpose(pT[:, j * 128:(j + 1) * 128], src[:, (g0 + j) * 128:(g0 + j + 1) * 128], identb[:])
                return ins
            P.op("pe", fn, reads=[Bsrc, Bc], writes=[BpT])
            for j in range(n):
                if j % 2:
                    P.op("act", lambda e, j=j, g0=g0: e.copy(out=dst_fn(g0 + j), in_=pT[:, j * 128:(j + 1) * 128]), reads=[BpT], writes=[Bdst])
                else:
                    P.op("dve", lambda e, j=j, g0=g0: e.tensor_copy(out=dst_fn(g0 + j), in_=pT[:, j * 128:(j + 1) * 128]), reads=[BpT], writes=[Bdst])

    def rms_to_T(xin, Bxin, xnorm, Bxnorm, Gs, Hs, dstb, Bdstb, router=False):
        P.op("act", lambda e: e.activation(out=rotm[:], in_=xin[:], func=AF.Square, accum_out=st1[:, 0:1]),
             reads=[Bxin], writes=[Brotm, Bst])
        P.op("dve", lambda e: e.tensor_scalar(out=st1[:, 1:2], in0=st1[:, 0:1], scalar1=1.0 / D, scalar2=EPS, op0=ALU.mult, op1=ALU.add),
             writes=[Bst])
        P.op("act", lambda e: e.activation(out=st1[:, 3:4], in_=st1[:, 1:2], func=AF.Sqrt), writes=[Bst])
        P.op("dve", lambda e: e.reciprocal(out=st1[:, 2:3], in_=st1[:, 3:4]), writes=[Bst])
        P.op("dve", lambda e: e.tensor_scalar(out=xnorm[:], in0=xin[:], scalar1=st1[:, 2:3], scalar2=None, op0=ALU.mult),
             reads=[Bxin, Bst], writes=[Bxnorm])
        for g0 in range(0, KC, 4):
            def fn(e, g0=g0):
                ins = None
                for j in range(4):
                    ins = e.transpose(pF[:, j * 128:(j + 1) * 128], xnorm[:, (g0 + j) * 128:(g0 + j + 1) * 128], identf[:])
                return ins
            P.op("pe", fn, reads=[Bxnorm, Bc], writes=[BpF])
            for j in range(4):
                kc = g0 + j
                P.op("act", lambda e, j=j, kc=kc: e.activation(out=dstb[:, kc, :], in_=pF[:, j * 128:(j + 1) * 128], func=AF.Identity,
                                                              bias=Hs[:, kc:kc + 1], scale=Gs[:, kc:kc + 1]),
                     reads=[BpF, Bmod], writes=[Bdstb])
                if router:
                    P.op("act", lambda e, j=j, kc=kc: e.activation(out=h32g[:, j, :], in_=pF[:, j * 128:(j + 1) * 128], func=AF.Identity,
                                                                  bias=Hs[:, kc:kc + 1], scale=Gs[:, kc:kc + 1]),
                         reads=[BpF, Bmod], writes=[Bh32g])
            if router:
                def fn(e, g0=g0):
                    ins = None
                    for j in range(4):
                        kc = g0 + j
                        ins = e.matmul(pS[:, 0:64], h32g[:, j, :], wr_sb[:, kc, :], start=(kc == 0), stop=(kc == KC - 1))
                    return ins
                P.op("pe", fn, reads=[Bh32g, Bc], writes=[BpS])

    def wcols(key, c0, n=512):
        return W[key][:, c0:c0 + n]

    def cut(n, buf, B):
        if cutn == n:
            P.op("sp", lambda e: e.dma_start(out=out_d[0:128, :], in_=buf[:]), reads=[B], dma="out")
            raise _Cut()

    def _body():
        cut(1, gbc, Bgbc)
        for tg in range(nseq):
            cur = tg % 2
            prv = 1 - cur
            hl = (hT, BhT, slice(0, 128))
            prefix = tg < main0
            tl = tg - main0
            P.op("sp", lambda e, tg=tg: e.dma_start(out=XA[:], in_=x_seq[tg * 128:(tg + 1) * 128, :]), writes=[BXA], dma="xa")
            if prefix:
                rms_to_T(XA, BXA, XB, BXB, G1f, SH1f, hT, BhT)
            else:
                rms_to_T(XA, BXA, XB, BXB, G1, SH1, hT, BhT)
            cut(2, XB, BXB)
            if prefix and tg != main0 - 1:
                pass
            else:
              pp, Bpp = nextA()
              proj([(pp, Bpp)], [hl], wcols("w_in", 2048), KC, key=("in", 4))
              P.op("act", lambda e, pp=pp: e.copy(out=kvtm[:], in_=pp[:, 0:256]), reads=[Bpp], writes=[Bkvtm])
              cut(311, XB, BXB)
              for g in range(4):
                  P.op("act", lambda e, pp=pp, cur=cur, g=g: e.copy(out=vau[cur][:, g, 0:64], in_=pp[:, 256 + g * 64:256 + (g + 1) * 64]),
                       reads=[Bpp], writes=[Bvau[cur]])
              cut(312, XB, BXB)

              def fn(e):
                  ins = None
                  for g in range(4):
                      ins = e.transpose(pT[0:64, g * 128:(g + 1) * 128], kvtm[:, g * 64:(g + 1) * 64], identb[:])
                  return ins
              P.op("pe", fn, reads=[Bkvtm, Bc], writes=[BpT])
              cut(313, XB, BXB)
              P.op("dve", lambda e, cur=cur: e.tensor_copy(out=kaT[cur][:].rearrange("p g t -> p (g t)"), in_=pT[0:64, 0:512]),
                   reads=[BpT], writes=[BkaT[cur]])
            cut(31, XB, BXB)
            dmt = dm0 if tg == 0 else (dmx if tg == main0 else dmg)
            for g in (range(4) if not prefix else ()):
                pp, Bpp = nextA()
                proj([(pp, Bpp)], [hl], wcols("w_in", g * 512), KC, key=("in", g))
                P.op("act", lambda e, pp=pp: e.copy(out=qtm[:], in_=pp[:]), reads=[Bpp], writes=[Bqtm])

                def fn(e):
                    ins = None
                    for hh_ in range(8):
                        ins = e.transpose(pT[0:64, hh_ * 128:(hh_ + 1) * 128], qtm[:, hh_ * 64:(hh_ + 1) * 64], identb[:])
                    return ins
                P.op("pe", fn, reads=[Bqtm, Bc], writes=[BpT])
                P.op("dve", lambda e: e.tensor_copy(out=qT[:].rearrange("p h t -> p (h t)"), in_=pT[0:64, 0:1024]), reads=[BpT], writes=[BqT])
                cut(32, XB, BXB)
                for hh_ in range(8):
                    hq = g * 8 + hh_

                    def fn(e, hh_=hh_, g=g, prv=prv, cur=cur):
                        e.matmul(pS[:, 0:128], kaT[prv][:, g, :], qT[:, hh_, :], start=True, stop=True)
                        return e.matmul(pS[:, 128:256], kaT[cur][:, g, :], qT[:, hh_, :], start=True, stop=True)
                    P.op("pe", fn, reads=[BkaT[0], BkaT[1], BqT], writes=[BpS])
                    P.op("dve", lambda e, hq=hq, dmt=dmt: e.scalar_tensor_tensor(out=sT[:], in0=dmt[:], scalar=-slopes[hq], in1=pS[:, 0:256],
                                                                                 op0=ALU.mult, op1=ALU.add), reads=[BpS, Bc], writes=[BsT])
                    P.op("act", lambda e: e.activation(out=eT[:], in_=sT[:], func=AF.Exp, scale=0.125), reads=[BsT], writes=[BeT])
                    cut(33, XB, BXB)

                    def fn(e, g=g, prv=prv, cur=cur):
                        e.matmul(pO[:, 0:66], eT[:, 0:128], vau[prv][:, g, 0:66], start=True, stop=False)
                        return e.matmul(pO[:, 0:66], eT[:, 128:256], vau[cur][:, g, 0:66], start=False, stop=True)
                    P.op("pe", fn, reads=[BeT, Bvau[0], Bvau[1]], writes=[BpO])
                    P.op("dve", lambda e, hq=hq: e.tensor_tensor(out=rden[:, 0:1], in0=pO[:, 64:65], in1=sinkexp[:, hq:hq + 1], op=ALU.add),
                         reads=[BpO, Bc], writes=[Brden])
                    P.op("dve", lambda e: e.reciprocal(out=rden[:, 1:2], in_=rden[:, 0:1]), writes=[Brden])
                    P.op("dve", lambda e, hq=hq: e.tensor_scalar(out=rotm[:, hq * 64:(hq + 1) * 64], in0=pO[:, 0:64], scalar1=rden[:, 1:2],
                                                                 scalar2=None, op0=ALU.mult), reads=[BpO, Brden], writes=[Brotm])
                    cut(34, XB, BXB)
            if not prefix:
                transposes_bf(rotm, Brotm, 2048, lambda c: aoT[:, c, :], BaoT)
            cut(3, XB, BXB)
            if prefix:
                for hp in range(4):
                    pp, Bpp = nextA()
                    proj([(pp, Bpp)], [hl], wcols("w_in", (9 + hp) * 512), KC, key=("in", 9 + hp))
                    P.op("act", lambda e, pp=pp: e.mul(out=rk[:], in_=pp[:], mul=1.0 / 16.0), reads=[Bpp], writes=[Brk])
                    for h2 in range(2):
                        h = hp * 2 + h2
                        c0 = h2 * 256
                        pp, Bpp = nextA()
                        proj([(pp, Bpp)], [hl], wcols("w_in", (13 + h) * 512), KC, key=("in", 13 + h))
                        P.op("act", lambda e, pp=pp: e.copy(out=rv[:], in_=pp[:]), reads=[Bpp], writes=[Brv])
                        P.op("dve", lambda e, c0=c0, h=h: e.tensor_scalar(out=rkd[:], in0=rk[:, c0:c0 + 256], scalar1=kdec[:, h:h + 1], scalar2=None,
                                                                         op0=ALU.mult), reads=[Brk, Bc], writes=[Brkd])
                        for dc in range(2):
                            P.op("pe", lambda e, dc=dc: e.matmul(pU[dc][:], rkd[:, dc * 128:(dc + 1) * 128], rv[:], start=True, stop=True),
                                 reads=[Brkd, Brv], writes=[BpU[dc]])
                            P.op("dve", lambda e, dc=dc, h=h: e.scalar_tensor_tensor(out=S32[:, h, dc, :], in0=S32[:, h, dc, :], scalar=gamC[h],
                                                                                    in1=pU[dc][:], op0=ALU.mult, op1=ALU.add),
                                 reads=[BpU[dc]], writes=[BS32[h]])
                if tg == 0 or tg == main0 - 1:
                    P.barrier()
                continue
            for hp in range(4):
                pp, Bpp = nextA()
                proj([(pp, Bpp)], [hl], wcols("w_in", (5 + hp) * 512), KC, key=("in", 5 + hp))
                P.op("act", lambda e, pp=pp: e.copy(out=rq[:], in_=pp[:]), reads=[Bpp], writes=[Brq])
                pp, Bpp = nextA()
                proj([(pp, Bpp)], [hl], wcols("w_in", (9 + hp) * 512), KC, key=("in", 9 + hp))
                P.op("act", lambda e, pp=pp: e.mul(out=rk[:], in_=pp[:], mul=1.0 / 16.0), reads=[Bpp], writes=[Brk])
                for h2 in range(2):
                    h = hp * 2 + h2
                    c0 = h2 * 256
                    pp, Bpp = nextA()
                    proj([(pp, Bpp)], [hl], wcols("w_in", (13 + h) * 512), KC, key=("in", 13 + h))
                    P.op("act", lambda e, pp=pp: e.copy(out=rv[:], in_=pp[:]), reads=[Bpp], writes=[Brv])
                    pp, Bpp = nextA()
                    proj([(pp, Bpp)], [hl], wcols("w_in", (21 + h) * 512), KC, key=("in", 21 + h))
                    P.op("act", lambda e, pp=pp: e.activation(out=rg[:], in_=pp[:], func=AF.Silu), reads=[Bpp], writes=[Brg])

                    def fn(e, c0=c0):
                        e.transpose(pT[:, 0:128], rq[:, c0:c0 + 128], identb[:])
                        e.transpose(pT[:, 128:256], rq[:, c0 + 128:c0 + 256], identb[:])
                        e.transpose(pT[:, 256:384], rk[:, c0:c0 + 128], identb[:])
                        return e.transpose(pT[:, 384:512], rk[:, c0 + 128:c0 + 256], identb[:])
                    P.op("pe", fn, reads=[Brq, Brk, Bc], writes=[BpT])
                    P.op("act", lambda e: e.copy(out=rqT[:].rearrange("p a b -> p (a b)"), in_=pT[:, 0:256]), reads=[BpT], writes=[BrqT])
                    for a_ in range(2):
                        P.op("dve", lambda e, h=h, a_=a_: e.tensor_tensor(out=rqdT[:, a_, :], in0=pT[:, a_ * 128:(a_ + 1) * 128],
                                                                         in1=qd[:, h, :], op=ALU.mult),
                             reads=[BpT, Bc], writes=[BrqdT])
                    P.op("act", lambda e: e.copy(out=rkT[:].rearrange("p a b -> p (a b)"), in_=pT[:, 256:512]), reads=[BpT], writes=[BrkT])
                    P.op("dve", lambda e, c0=c0, h=h: e.tensor_scalar(out=rkd[:], in0=rk[:, c0:c0 + 256], scalar1=kdec[:, h:h + 1], scalar2=None,
                                                                     op0=ALU.mult), reads=[Brk, Bc], writes=[Brkd])
                    P.op("act", lambda e, h=h: e.copy(out=Sbfh[:].rearrange("p a b -> p (a b)"), in_=S32[:, h].rearrange("p a b -> p (a b)")),
                         reads=[BS32[h]], writes=[BSbfh])

                    def fn(e):
                        e.matmul(pS[:, 0:128], rkT[:, 0, :], rqT[:, 0, :], start=True, stop=False)
                        return e.matmul(pS[:, 0:128], rkT[:, 1, :], rqT[:, 1, :], start=False, stop=True)
                    P.op("pe", fn, reads=[BrkT, BrqT], writes=[BpS])
                    P.op("dve", lambda e, h=h: e.tensor_tensor(out=adT[:], in0=pS[:, 0:128], in1=dmask[:, h, :], op=ALU.mult),
                         reads=[BpS, Bc], writes=[BadT])

                    def fn(e):
                        e.matmul(pO[:], adT[:], rv[:], start=True, stop=False)
                        e.matmul(pO[:], rqdT[:, 0, :], Sbfh[:, 0, :], start=False, stop=False)
                        return e.matmul(pO[:], rqdT[:, 1, :], Sbfh[:, 1, :], start=False, stop=True)
                    P.op("pe", fn, reads=[BadT, Brv, BrqdT, BSbfh], writes=[BpO])
                    for dc in range(2):
                        P.op("pe", lambda e, dc=dc: e.matmul(pU[dc][:], rkd[:, dc * 128:(dc + 1) * 128], rv[:], start=True, stop=True),
                             reads=[Brkd, Brv], writes=[BpU[dc]])
                        P.op("dve", lambda e, dc=dc, h=h: e.scalar_tensor_tensor(out=S32[:, h, dc, :], in0=S32[:, h, dc, :], scalar=gamC[h],
                                                                                in1=pU[dc][:], op0=ALU.mult, op1=ALU.add),
                             reads=[BpU[dc]], writes=[BS32[h]])
                    P.op("dve", lambda e: e.bn_stats(out=bnst[:], in_=pO[:]), reads=[BpO], writes=[Bbn])
                    P.op("dve", lambda e: e.bn_aggr(out=bnag[:], in_=bnst[:]), writes=[Bbn])
                    P.op("dve", lambda e: e.tensor_scalar(out=bnag[:, 1:2], in0=bnag[:, 1:2], scalar1=EPS, scalar2=None, op0=ALU.add), writes=[Bbn])
                    P.op("act", lambda e: e.activation(out=bnag[:, 1:2], in_=bnag[:, 1:2], func=AF.Sqrt), writes=[Bbn])
                    P.op("dve", lambda e: e.reciprocal(out=bnag[:, 1:2], in_=bnag[:, 1:2]), writes=[Bbn])
                    P.op("dve", lambda e: e.tensor_scalar(out=otmp[:], in0=pO[:], scalar1=bnag[:, 0:1], scalar2=bnag[:, 1:2],
                                                          op0=ALU.subtract, op1=ALU.mult), reads=[BpO, Bbn], writes=[Botmp])
                    P.op("dve", lambda e, h=h: e.tensor_tensor(out=rotm[:, h * 512:(h + 1) * 512], in0=otmp[:], in1=rg[:], op=ALU.mult),
                         reads=[Botmp, Brg], writes=[Brotm])
            transposes_bf(rotm, Brotm, D, lambda c: roT[:, c, :], BroT)
            cut(4, XB, BXB)
            for cb in range(8):
                cs = slice(cb * 512, (cb + 1) * 512)
                pp, Bpp = nextA()
                proj([(pp, Bpp)], [hl], wcols("w_in", (29 + cb) * 512), KC, key=("in", 29 + cb))
                P.op("act", lambda e, pp=pp: e.activation(out=sga[:], in_=pp[:], func=AF.Sigmoid), reads=[Bpp], writes=[Bsga])
                pp, Bpp = nextA()
                proj([(pp, Bpp)], [(aoT, BaoT, slice(0, 128))], W["w_ao"][:, cs], 16, key=("ao", cb))
                P.op("dve", lambda e, pp=pp, cs=cs: e.tensor_tensor(out=XB[:, cs], in0=sga[:], in1=pp[:], op=ALU.mult), reads=[Bpp, Bsga], writes=[BXB])
                pp, Bpp = nextA()
                proj([(pp, Bpp)], [hl], wcols("w_in", (37 + cb) * 512), KC, key=("in", 37 + cb))
                P.op("act", lambda e, pp=pp: e.activation(out=sgb[:], in_=pp[:], func=AF.Sigmoid), reads=[Bpp], writes=[Bsgb])
                pp, Bpp = nextA()
                proj([(pp, Bpp)], [(roT, BroT, slice(0, 128))], W["w_ro"][:, cs], KC, key=("ro", cb))
                P.op("dve", lambda e, pp=pp: e.tensor_tensor(out=sgb[:], in0=sgb[:], in1=pp[:], op=ALU.mult), reads=[Bpp], writes=[Bsgb])
                P.op("dve", lambda e, cs=cs: e.tensor_tensor(out=rotm[:, cs], in0=sgb[:], in1=XB[:, cs], op=ALU.add), reads=[Bsgb, BXB], writes=[Brotm])
            transposes_bf(rotm, Brotm, D, lambda c: roT[:, c, :], BroT)
            cut(5, XB, BXB)
            for cb in range(8):
                cs = slice(cb * 512, (cb + 1) * 512)
                pp, Bpp = nextA()
                proj([(pp, Bpp)], [(roT, BroT, slice(0, 128))], W["w_o"][:, cs], KC, key=("o", cb))
                P.op("dve", lambda e, pp=pp, cs=cs: e.tensor_tensor(out=sga[:], in0=pp[:], in1=gbc[:, cs], op=ALU.mult), reads=[Bpp, Bgbc], writes=[Bsga])
                P.op("dve", lambda e, cs=cs: e.tensor_tensor(out=XA[:, cs], in0=XA[:, cs], in1=sga[:], op=ALU.add), reads=[Bsga], writes=[BXA])
            if not full:
                P.op("sp", lambda e, tl=tl: e.dma_start(out=out_d[tl * 128:(tl + 1) * 128, :], in_=XA[:]), reads=[BXA], writes=[Bx1d], dma="out")
                if tg == main0 and nseq > main0 + 1:
                    P.barrier()
                continue
            P.op("sp", lambda e, tl=tl: e.dma_start(out=x1_d[tl * 128:(tl + 1) * 128, :], in_=XA[:]), reads=[BXA], writes=[Bx1d], dma="x1o")
            rms_to_T(XA, BXA, XB, BXB, G2, SH2, hT, BhT, router=True)
            P.op("sp", lambda e, tl=tl: e.dma_start(out=h2T_d[:, :, tl * 128:(tl + 1) * 128], in_=hT[:]), reads=[BhT], writes=[Bh2d], dma="h2o")
            sc, ch, t1, t2, cm, rw = rt["sc"], rt["ch"], rt["t1"], rt["t2"], rt["cm"], rt["rw"]
            m1, m2, gs, top, gm, mx = rt8["m1"], rt8["m2"], rt8["gs"], rt8["top"], rt8["gm"], rt8["mx"]
            R = dict(reads=[Bc], writes=[Brt])
            P.op("act", lambda e: e.activation(out=sc[:], in_=pS[:, 0:64], func=AF.Sigmoid), reads=[BpS], writes=[Brt])
            P.op("dve", lambda e: e.tensor_tensor(out=ch[:], in0=sc[:], in1=brbc[:], op=ALU.add), **R)
            ch3 = ch[:].rearrange("p (g k) -> p g k", g=8)
            P.op("dve", lambda e: e.tensor_reduce(out=m1[:], in_=ch3, axis=AX.X, op=ALU.max), **R)
            P.op("dve", lambda e: e.tensor_tensor(out=t1[:].rearrange("p (g k) -> p g k", g=8), in0=ch3,
                                                  in1=m1[:].unsqueeze(2).to_broadcast([128, 8, 8]), op=ALU.is_equal), **R)
            P.op("dve", lambda e: e.scalar_tensor_tensor(out=t2[:], in0=t1[:], scalar=-1.0e9, in1=ch[:], op0=ALU.mult, op1=ALU.add), **R)
            P.op("dve", lambda e: e.tensor_reduce(out=m2[:], in_=t2[:].rearrange("p (g k) -> p g k", g=8), axis=AX.X, op=ALU.max), **R)
            P.op("dve", lambda e: e.tensor_tensor(out=gs[:], in0=m1[:], in1=m2[:], op=ALU.add), **R)
            P.op("dve", lambda e: e.max(out=top[:], in_=gs[:]), **R)
            P.op("dve", lambda e: e.tensor_scalar(out=gm[:], in0=gs[:], scalar1=top[:, 3:4], scalar2=None, op0=ALU.is_ge), **R)
            P.op("dve", lambda e: e.tensor_scalar(out=gm[:], in0=gm[:], scalar1=-1.0, scalar2=1.0e9, op0=ALU.add, op1=ALU.mult), **R)
            P.op("dve", lambda e: e.tensor_tensor(out=cm[:].rearrange("p (g k) -> p g k", g=8), in0=ch3,
                                                  in1=gm[:].unsqueeze(2).to_broadcast([128, 8, 8]), op=ALU.add), **R)
            P.op("dve", lambda e: e.max(out=mx[:], in_=cm[:]), **R)
            P.op("dve", lambda e: e.tensor_scalar(out=t1[:], in0=cm[:], scalar1=mx[:, 7:8], scalar2=None, op0=ALU.is_ge), **R)
            P.op("dve", lambda e: e.tensor_tensor(out=t2[:], in0=t1[:], in1=sc[:], op=ALU.mult), **R)
            P.op("dve", lambda e: e.tensor_reduce(out=m1[:, 0:1], in_=t2[:], axis=AX.X, op=ALU.add), **R)
            P.op("dve", lambda e: e.reciprocal(out=m1[:, 1:2], in_=m1[:, 0:1]), **R)
            P.op("dve", lambda e: e.tensor_scalar(out=rw[:], in0=t2[:], scalar1=m1[:, 1:2], scalar2=2.5, op0=ALU.mult, op1=ALU.mult), **R)
            P.op("sp", lambda e, tl=tl: e.dma_start(out=rw_d[:, tl, :], in_=rw[:]), reads=[Brt], writes=[Brwd], dma="rwo")
            if tg == main0 and nseq > main0 + 1:
                P.barrier()

        if full:
            P.barrier()
            S32v = S32[:].rearrange("p h a b -> p (h a b)")
            X1L = S32v[:, 0:D]; GF = S32v[:, D:2 * D]
            BX1L = Buf("X1L"); BGF = Buf("GF")
            yacc = [XA, XB]; Byacc = [Buf("yacc0"), Buf("yacc1")]
            h2s = [hT, roT]; Bh2s = [Buf("h2s0"), Buf("h2s1")]
            hh = sb("hh", [128, 512], BF16); Bhh = Buf("hh")
            hhT = [sb("hhT%d" % j, [128, 4, 128], BF16) for j in range(2)]; BhhT = [Buf("hhT%d" % j) for j in range(2)]
            RW2 = sb("RW2", [128, 2, 64]); BRW2 = Buf("RW2")
            ones1 = sb("ones1", [128, 1]); Bones = Buf("ones1")
            P.op("dve", lambda e: e.memset(ones1[:], 1.0), writes=[Bones])
            Bgbc2 = Buf("gbc2")
            P.op("sp", lambda e: e.dma_start(out=gbc[:], in_=mod_d[:, 5 * D:6 * D].partition_broadcast(128)), writes=[Bgbc2], dma="gbc")
            P.op("sp", lambda e: e.dma_start(out=GF, in_=b_ada_in[:, 5 * D:6 * D].partition_broadcast(128)), writes=[BGF], dma="gf")
            P.op("dve", lambda e: e.tensor_tensor(out=gbc[:], in0=gbc[:], in1=GF, op=ALU.add), reads=[BGF], writes=[Bgbc2])
            P.op("sp", lambda e: e.dma_start(out=GF, in_=g_fin_in.partition_broadcast(128)), reads=[Bgbc2], writes=[BGF], dma="gf")
            gsrc = [(pA[0], BpA[0]), (pA[1], BpA[1])]
            usrc = [(pS, BpS), (pO, BpO)]
            for ps_ in range(nmain // 2):
                for j in range(2):
                    tg = ps_ * 2 + j
                    P.op("sp", lambda e, j=j, tg=tg: e.dma_start(out=h2s[j][:], in_=h2T_d[:, :, tg * 128:(tg + 1) * 128]), writes=[Bh2s[j]], dma="mh%d" % j)
                    P.op("dve", lambda e, j=j: e.memset(yacc[j][:], 0.0), writes=[Byacc[j]])
                P.op("sp", lambda e, ps_=ps_: e.dma_start(out=RW2[:], in_=rw_d[:, ps_ * 2:ps_ * 2 + 2, :]), writes=[BRW2], dma="rw2")
                lhs = [(h2s[0], Bh2s[0], slice(0, 128)), (h2s[1], Bh2s[1], slice(0, 128))]
                for ex in range(65):
                    if ex < 64:
                        sg_, su_, sd_ = W["w_gate"][ex], W["w_up"][ex], W["w_down"][ex]
                    else:
                        sg_, su_, sd_ = W["w_sg"], W["w_su"], W["w_sd"]
                    proj(gsrc, lhs, sg_, KC, key=("g", ex))
                    proj(usrc, lhs, su_, KC, key=("u", ex))
                    for j in range(2):
                        pg, Bpg = gsrc[j]
                        pu, Bpu = usrc[j]
                        P.op("act", lambda e, pg=pg: e.activation(out=sga[:], in_=pg[:], func=AF.Silu), reads=[Bpg], writes=[Bsga])
                        wsc = RW2[:, j, ex:ex + 1] if ex < 64 else ones1[:, 0:1]
                        P.op("dve", lambda e, pu=pu, wsc=wsc: e.scalar_tensor_tensor(out=hh[:], in0=sga[:], scalar=wsc, in1=pu[:], op0=ALU.mult, op1=ALU.mult),
                             reads=[Bsga, Bpu, BRW2, Bones], writes=[Bhh])

                        def fn(e):
                            ins = None
                            for m in range(4):
                                ins = e.transpose(pT[:, m * 128:(m + 1) * 128], hh[:, m * 128:(m + 1) * 128], identb[:])
                            return ins
                        P.op("pe", fn, reads=[Bhh, Bc], writes=[BpT])
                        P.op("act", lambda e, j=j: e.copy(out=hhT[j][:].rearrange("p a b -> p (a b)"), in_=pT[:, 0:512]), reads=[BpT], writes=[BhhT[j]])
                    for cq in range(4):
                        wdt, Bwdt = load_raw(("d", ex, cq), lambda e, i, sd_=sd_, cq=cq: e.dma_start(
                            out=wb[i][:].rearrange("p (m c) n -> p m (c n)", m=4),
                            in_=sd_[:, cq * 1024:(cq + 1) * 1024].rearrange("(m p) n -> p m n", p=128)))
                        for j in range(2):
                            for c2 in range(2):
                                py, Bpy = pU[c2], BpU[c2]
                                cs = slice(cq * 1024 + c2 * 512, cq * 1024 + (c2 + 1) * 512)

                                def fn(e, py=py, c2=c2, wdt=wdt, j=j):
                                    ins = None
                                    for m in range(4):
                                        ins = e.matmul(py[:], hhT[j][:, m, :], wdt[:, m * 2 + c2, :], start=(m == 0), stop=(m == 3))
                                    return ins
                                P.op("pe", fn, reads=[BhhT[j], Bwdt], writes=[Bpy])
                                P.op("dve", lambda e, py=py, cs=cs, j=j: e.tensor_tensor(out=yacc[j][:, cs], in0=yacc[j][:, cs], in1=py[:], op=ALU.add),
                                     reads=[Bpy], writes=[Byacc[j]])
                for j in range(2):
                    tg = ps_ * 2 + j
                    P.op("sp", lambda e, tg=tg: e.dma_start(out=X1L, in_=x1_d[tg * 128:(tg + 1) * 128, :]), writes=[BX1L], dma="x1i")
                    P.op("dve", lambda e, j=j: e.tensor_tensor(out=yacc[j][:], in0=yacc[j][:], in1=gbc[:], op=ALU.mult), reads=[Bgbc2], writes=[Byacc[j]])
                    P.op("dve", lambda e, j=j: e.tensor_tensor(out=yacc[j][:], in0=yacc[j][:], in1=X1L, op=ALU.add), reads=[BX1L], writes=[Byacc[j]])
                    P.op("act", lambda e, j=j: e.activation(out=rotm[:], in_=yacc[j][:], func=AF.Square, accum_out=st1[:, 0:1]),
                         reads=[Byacc[j]], writes=[Brotm, Bst])
                    P.op("dve", lambda e: e.tensor_scalar(out=st1[:, 1:2], in0=st1[:, 0:1], scalar1=1.0 / D, scalar2=EPS, op0=ALU.mult, op1=ALU.add), writes=[Bst])
                    P.op("act", lambda e: e.activation(out=st1[:, 3:4], in_=st1[:, 1:2], func=AF.Sqrt), writes=[Bst])
                    P.op("dve", lambda e: e.reciprocal(out=st1[:, 2:3], in_=st1[:, 3:4]), writes=[Bst])
                    P.op("dve", lambda e, j=j: e.scalar_tensor_tensor(out=X1L, in0=yacc[j][:], scalar=st1[:, 2:3], in1=GF, op0=ALU.mult, op1=ALU.mult),
                         reads=[Byacc[j], Bst, BGF], writes=[BX1L])
                    P.op("sp", lambda e, tg=tg: e.dma_start(out=out_d[tg * 128:(tg + 1) * 128, :], in_=X1L), reads=[BX1L], dma="out")
                if ps_ == 0 and nmain > 2:
                    P.barrier()
    try:
        _body()
    except _Cut:
        pass
    P.barrier()
    P.emit()
    es.close()
    return nc


def _tables():
    q = np.arange(128)[None, :]
    k = np.arange(128)[:, None]
    dcur = (q - k).astype(np.float32)
    dprev = (q + 128 - k).astype(np.float32)
    cur = np.where(dcur >= 0, 8.0 * dcur, BIG).astype(np.float32)
    prev = np.where(dprev < 128, 8.0 * dprev, BIG).astype(np.float32)
    dmg = np.concatenate([prev, cur], axis=1)
    dm0 = np.concatenate([np.full_like(prev, BIG), cur], axis=1)
    gam = 1.0 - 2.0 ** (-5.0 - np.arange(8, dtype=np.float64))
    i = np.arange(128)
    dmask = np.zeros((128, 8, 128), np.float32)
    qd = np.zeros((128, 8, 128), np.float32)
    kdec = np.zeros((128, 8), np.float32)
    for h in range(8):
        rel = i[None, :] - i[:, None]
        dmask[:, h, :] = np.where(rel >= 0, gam[h] ** np.maximum(rel, 0), 0.0)
        qd[:, h, :] = (gam[h] ** (i + 1.0))[None, :]
        kdec[:, h] = gam[h] ** (127.0 - i)
    return dmg, dm0, dmask.reshape(128, -1), qd.reshape(128, -1), kdec


def make_in_map(inp, xs, cvec, full=True, half=1, first_is_seq_start=True):
    dmg, dm0, dmask, qd, kdec = _tables()
    f32 = lambda a: np.ascontiguousarray(np.asarray(a, np.float32))
    fm = lambda v, n=KC: np.ascontiguousarray(np.asarray(v, np.float32).reshape(n, 128).T)
    m = {
        "x_seq": f32(xs), "c_fm": fm(cvec), "dm0": dm0, "dmg": dmg, "dmask": dmask, "qd": qd, "kdec": kdec,
        "dmx": (dm0 if first_is_seq_start else dmg), "flag": np.full((128, 1), float(half), np.float32),
        "identb": np.eye(128, dtype=np.float32).astype(ml_dtypes.bfloat16), "identf": np.eye(128, dtype=np.float32),
        "b_ada": f32(inp["b_ada"]).reshape(1, -1), "b_ada_fm": fm(inp["b_ada"], 6 * KC),
        "g_mix_fm": fm(inp["g_norm_mix"]), "g_ffn_fm": fm(inp["g_norm_ffn"]),
        "g_fin": f32(inp["g_norm_final"]).reshape(1, -1), "sinks": f32(inp["attn_sinks"]).reshape(1, -1),
        "b_router": f32(inp["b_router"]).reshape(1, -1),
        "w_ada": f32(inp["w_ada"][0]), "w_in": f32(inp["w_in"][0]), "w_ao": f32(inp["w_attn_out"][0]),
        "w_ro": f32(inp["w_ret_out"][0]), "w_o": f32(inp["w_o"][0]), "w_router": f32(inp["w_router"][0]),
    }
    if full:
        m.update({"w_gate": f32(inp["w_gate"][0]), "w_up": f32(inp["w_up"][0]), "w_down": f32(inp["w_down"][0]),
                  "w_sg": f32(inp["w_sh_gate"][0]), "w_su": f32(inp["w_sh_up"][0]), "w_sd": f32(inp["w_sh_down"][0])})
    return m


def kernel(**inp):
    x = np.asarray(inp["x"], np.float32)
    c = np.asarray(inp["c"], np.float32)
    ncore = 8
    nc = build_program(32, "full", 0, 16)
    in_maps = []
    for core in range(ncore):
        b, half = core // 2, core % 2
        if half:
            xs = x[b]
        else:
            xs = np.concatenate([np.zeros((2048, D), np.float32), x[b, :2048]], axis=0)
        in_maps.append(make_in_map(inp, xs, c[b], half=half, first_is_seq_start=(half == 0)))
    res = run_bass_kernel_spmd(nc, in_maps, core_ids=list(range(ncore)))
    out = np.empty((4, 4096, D), np.float32)
    for core in range(ncore):
        b, half = core // 2, core % 2
        out[b, half * 2048:(half + 1) * 2048] = res.results[core]["out"]
    return out
```
